# Optimizing a Trainium2 kernel written in Bass

```python
import jax, jax.numpy as jnp
from jax import lax
import numpy as np

D_MODEL = 1024
BATCH = 16
SEQ = 2048
DEPTH = 2
DEC_BATCH = 128
DEC_SEQ = 4
PAST_LEN = 16384
PAGE_SIZE = 128

RET_HEADS = 4
RET_QK_DIM = D_MODEL // RET_HEADS
RET_V_DIM = 2 * RET_QK_DIM
RET_QK_W = RET_HEADS * RET_QK_DIM
RET_V_W = RET_HEADS * RET_V_DIM
RET_CHUNK = 128
RET_ROPE_THETA = 10000.0
SWA_HEAD_DIM = 64
SWA_Q_HEADS = D_MODEL // SWA_HEAD_DIM
SWA_KV_HEADS = 4
SWA_GROUP = SWA_Q_HEADS // SWA_KV_HEADS
WINDOW = 128
SWA_BLOCK = WINDOW
ROPE_THETA = 500000.0
ROT_DIM = SWA_HEAD_DIM // 4
D_FF = 4 * D_MODEL
EPS = 1e-6
NEG = -1e30

kernel_name = "yoco_retention_swa_sink_decoder_step"


def rms_norm(x, g):
    xf = x.astype(jnp.float32)
    y = xf * lax.rsqrt(jnp.mean(xf * xf, axis=-1, keepdims=True) + EPS)
    return y.astype(x.dtype) * g.astype(x.dtype)


def rope(x, pos, inv_freq):
    half = inv_freq.shape[0]
    rot = 2 * half
    ang = pos[:, None] * inv_freq[None, :]
    cos = jnp.cos(ang)[None, :, None, :].astype(x.dtype)
    sin = jnp.sin(ang)[None, :, None, :].astype(x.dtype)
    x1 = x[..., :half]
    x2 = x[..., half:rot]
    return jnp.concatenate([x1 * cos - x2 * sin, x2 * cos + x1 * sin, x[..., rot:]], axis=-1)


def ret_inv_freq():
    return 1.0 / (RET_ROPE_THETA ** jnp.linspace(0.0, 1.0, RET_QK_DIM // 2, dtype=jnp.float32))


def partial_inv_freq():
    half = ROT_DIM // 2
    return ROPE_THETA ** (-jnp.arange(half, dtype=jnp.float32) / half)


def retention_chunkwise(q, k, v, state0, chunk):
    B, T, H, dk = q.shape
    n = T // chunk
    log_g = jnp.log1p(-jnp.exp2(-5.0 - jnp.arange(H, dtype=jnp.float32)))
    idx = jnp.arange(chunk, dtype=jnp.float32)
    rel = idx[:, None] - idx[None, :]
    intra = jnp.where(rel[None] >= 0, jnp.exp(log_g[:, None, None] * jnp.maximum(rel, 0.0)[None]), 0.0)
    q_decay = jnp.exp(log_g[:, None] * (idx[None, :] + 1.0))
    k_decay = jnp.exp(log_g[:, None] * (chunk - 1.0 - idx[None, :]))
    chunk_decay = jnp.exp(log_g * chunk)[None, :, None, None]

    def to_chunks(a):
        return jnp.moveaxis(a.reshape(B, n, chunk, H, a.shape[-1]), 1, 0)

    def step(S, qkv):
        qc, kc, vc = qkv
        sc = jnp.einsum('bihd,bjhd->bhij', qc, kc) * intra[None]
        o = jnp.einsum('bhij,bjhe->bihe', sc, vc)
        o = o + jnp.einsum('bihd,hi,bhde->bihe', qc, q_decay, S)
        S = chunk_decay * S + jnp.einsum('bjhd,hj,bjhe->bhde', kc, k_decay, vc)
        return S, o

    S, o = lax.scan(step, state0, (to_chunks(q), to_chunks(k), to_chunks(v)))
    o = jnp.moveaxis(o, 0, 1).reshape(B, T, H, v.shape[-1])
    return o, S


def retention_layer(h, pos, state0, g_pre, w_in, w_out, g_post):
    B, T, _ = h.shape
    x = rms_norm(h, g_pre)
    proj = x @ w_in
    q = proj[..., :RET_QK_W].reshape(B, T, RET_HEADS, RET_QK_DIM)
    k = proj[..., RET_QK_W:2 * RET_QK_W].reshape(B, T, RET_HEADS, RET_QK_DIM)
    v = proj[..., 2 * RET_QK_W:2 * RET_QK_W + RET_V_W].reshape(B, T, RET_HEADS, RET_V_DIM)
    gate = proj[..., 2 * RET_QK_W + RET_V_W:]
    inv = ret_inv_freq()
    q = rope(q, pos, inv)
    k = rope(k, pos, inv) * (RET_QK_DIM ** -0.5)
    chunk = RET_CHUNK if T % RET_CHUNK == 0 else T
    o, S = retention_chunkwise(q.astype(jnp.float32), k.astype(jnp.float32),
                               v.astype(jnp.float32), state0.astype(jnp.float32), chunk)
    o = o * lax.rsqrt(jnp.mean(o * o, axis=-1, keepdims=True) + EPS)
    o = o.astype(h.dtype).reshape(B, T, RET_V_W) * jax.nn.silu(gate)
    return h + rms_norm(o @ w_out, g_post), S


def shared_kv(h, pos, g_kv, w_kv):
    B, T, _ = h.shape
    kv = (rms_norm(h, g_kv) @ w_kv).reshape(B, T, 2, SWA_KV_HEADS, SWA_HEAD_DIM)
    k = rope(kv[:, :, 0], pos, partial_inv_freq())
    return k, kv[:, :, 1]


def sink_attention(q, k, v, qpos, kpos, sinks):
    s = jnp.einsum('bnqkgd,bnskd->bnkgqs', q, k).astype(jnp.float32) * (SWA_HEAD_DIM ** -0.5)
    rel = qpos[:, :, None] - kpos[:, None, :]
    ok = (rel >= 0) & (rel < WINDOW) & (kpos[:, None, :] >= 0)
    s = jnp.where(ok[None, :, None, None], s, NEG)
    sk = sinks.astype(jnp.float32).reshape(SWA_KV_HEADS, SWA_GROUP)[None, None, :, :, None, None]
    sk = jnp.broadcast_to(sk, s.shape[:-1] + (1,))
    p = jax.nn.softmax(jnp.concatenate([s, sk], axis=-1), axis=-1)[..., :-1]
    return jnp.einsum('bnkgqs,bnskd->bnqkgd', p.astype(v.dtype), v)


def attend_prompt(q, k, v, sinks):
    B, T, H, hd = q.shape
    n = T // SWA_BLOCK
    qb = q.reshape(B, n, SWA_BLOCK, SWA_KV_HEADS, SWA_GROUP, hd)

    def band(a):
        ab = a.reshape(B, n, SWA_BLOCK, SWA_KV_HEADS, hd)
        prev = jnp.concatenate([jnp.zeros_like(ab[:, :1]), ab[:, :-1]], axis=1)
        return jnp.concatenate([prev, ab], axis=2)

    qpos = jnp.arange(T, dtype=jnp.int32).reshape(n, SWA_BLOCK)
    kpos = (jnp.arange(n, dtype=jnp.int32)[:, None] - 1) * SWA_BLOCK + jnp.arange(2 * SWA_BLOCK, dtype=jnp.int32)[None]
    o = sink_attention(qb, band(k), band(v), qpos, kpos, sinks)
    return o.reshape(B, T, H * hd)


def attend_sample(q, k_all, v_all, sinks, q_start):
    B, T, H, hd = q.shape
    S = k_all.shape[1]
    qpos = (q_start + jnp.arange(T, dtype=jnp.int32))[None]
    kpos = (q_start - (S - T) + jnp.arange(S, dtype=jnp.int32))[None]
    o = sink_attention(q.reshape(B, 1, T, SWA_KV_HEADS, SWA_GROUP, hd), k_all[:, None], v_all[:, None],
                       qpos, kpos, sinks)
    return o.reshape(B, T, H * hd)


def swa_layer(h, pos, attend, g_pre, w_q, w_o, g_post):
    B, T, _ = h.shape
    q = (rms_norm(h, g_pre) @ w_q).reshape(B, T, SWA_Q_HEADS, SWA_HEAD_DIM)
    q = rope(q, pos, partial_inv_freq())
    o = attend(q)
    return h + rms_norm(o @ w_o, g_post)


def ffn(h, g_pre, w1, w2, g_post):
    u = jax.nn.relu(rms_norm(h, g_pre) @ w1)
    return h + rms_norm((u * u) @ w2, g_post)


def setup_inputs(seed: int = 0) -> dict:
    key = jax.random.key(seed)
    ks = jax.random.split(key, 24)
    n_a = DEPTH // 2
    n_b = DEPTH - n_a
    w_cache = min(WINDOW, PAST_LEN)

    def nrm(k, shape, scale):
        return jax.random.normal(k, shape, jnp.float32) * scale

    def gain(k, shape):
        return 1.0 + 0.05 * jax.random.normal(k, shape, jnp.float32)

    w_in_width = 2 * RET_QK_W + 2 * RET_V_W
    kv_shape = (DEC_BATCH, w_cache, SWA_KV_HEADS, SWA_HEAD_DIM)
    return {
        "x_prompt": nrm(ks[0], (BATCH, SEQ, D_MODEL), 1.0),
        "x_sample": nrm(ks[1], (DEC_BATCH, DEC_SEQ, D_MODEL), 1.0),
        "state_ret": nrm(ks[2], (n_a, DEC_BATCH, RET_HEADS, RET_QK_DIM, RET_V_DIM), 0.5),
        "cache_k_win": nrm(ks[3], kv_shape, 1.0),
        "cache_v_win": nrm(ks[4], kv_shape, 1.0),
        "ret_norm_pre": gain(ks[5], (n_a, D_MODEL)),
        "ret_w_in": nrm(ks[6], (n_a, D_MODEL, w_in_width), D_MODEL ** -0.5),
        "ret_w_out": nrm(ks[7], (n_a, RET_V_W, D_MODEL), RET_V_W ** -0.5),
        "ret_norm_post": gain(ks[8], (n_a, D_MODEL)),
        "kv_norm": gain(ks[9], (D_MODEL,)),
        "w_kv": nrm(ks[10], (D_MODEL, 2 * SWA_KV_HEADS * SWA_HEAD_DIM), D_MODEL ** -0.5),
        "swa_norm_pre": gain(ks[11], (n_b, D_MODEL)),
        "swa_w_q": nrm(ks[12], (n_b, D_MODEL, SWA_Q_HEADS * SWA_HEAD_DIM), D_MODEL ** -0.5),
        "swa_sinks": nrm(ks[13], (n_b, SWA_Q_HEADS), 1.0),
        "swa_w_o": nrm(ks[14], (n_b, SWA_Q_HEADS * SWA_HEAD_DIM, D_MODEL), (SWA_Q_HEADS * SWA_HEAD_DIM) ** -0.5),
        "swa_norm_post": gain(ks[15], (n_b, D_MODEL)),
        "ffn_norm_pre": gain(ks[16], (DEPTH, D_MODEL)),
        "ffn_w1": nrm(ks[17], (DEPTH, D_MODEL, D_FF), D_MODEL ** -0.5),
        "ffn_w2": nrm(ks[18], (DEPTH, D_FF, D_MODEL), D_FF ** -0.5),
        "ffn_norm_post": gain(ks[19], (DEPTH, D_MODEL)),
    }


def reference(x_prompt, x_sample, state_ret, cache_k_win, cache_v_win,
              ret_norm_pre, ret_w_in, ret_w_out, ret_norm_post,
              kv_norm, w_kv,
              swa_norm_pre, swa_w_q, swa_sinks, swa_w_o, swa_norm_post,
              ffn_norm_pre, ffn_w1, ffn_w2, ffn_norm_post):
    n_a = DEPTH // 2

    def trunk(h, pos, ret_state0, make_attend):
        ret_states = []
        k = v = None
        attend = None
        for l in range(DEPTH):
            if l < n_a:
                h, S = retention_layer(h, pos, ret_state0[l], ret_norm_pre[l], ret_w_in[l],
                                       ret_w_out[l], ret_norm_post[l])
                ret_states.append(S)
            else:
                if l == n_a:
                    k, v = shared_kv(h, pos, kv_norm, w_kv)
                    attend = make_attend(k, v)
                b = l - n_a
                h = swa_layer(h, pos, functools_partial(attend, swa_sinks[b]), swa_norm_pre[b],
                              swa_w_q[b], swa_w_o[b], swa_norm_post[b])
            h = ffn(h, ffn_norm_pre[l], ffn_w1[l], ffn_w2[l], ffn_norm_post[l])
        return h, jnp.stack(ret_states), k, v

    def functools_partial(attend, sinks):
        return lambda q: attend(q, sinks)

    B_p, T_p, _ = x_prompt.shape
    pos_p = jnp.arange(T_p, dtype=jnp.float32)
    zero_state = jnp.zeros((n_a, B_p, RET_HEADS, RET_QK_DIM, RET_V_DIM), jnp.float32)
    y_prompt, state_ret_p, k_p, v_p = trunk(
        x_prompt, pos_p, zero_state,
        lambda k, v: (lambda q, sinks: attend_prompt(q, k, v, sinks)))
    w_p = min(WINDOW, T_p)
    k_win_p = k_p[:, T_p - w_p:]
    v_win_p = v_p[:, T_p - w_p:]

    B_s, T_s, _ = x_sample.shape
    pos_s = PAST_LEN + jnp.arange(T_s, dtype=jnp.float32)
    w_s = cache_k_win.shape[1]
    kv_all = {}

    def make_attend_sample(k, v):
        kv_all['k'] = jnp.concatenate([cache_k_win.astype(k.dtype), k], axis=1)
        kv_all['v'] = jnp.concatenate([cache_v_win.astype(v.dtype), v], axis=1)
        return lambda q, sinks: attend_sample(q, kv_all['k'], kv_all['v'], sinks, PAST_LEN)

    y_sample, state_ret_s, _, _ = trunk(x_sample, pos_s, state_ret, make_attend_sample)
    k_win_s = kv_all['k'][:, T_s:T_s + w_s]
    v_win_s = kv_all['v'][:, T_s:T_s + w_s]

    return (y_prompt, y_sample, state_ret_p, state_ret_s, k_win_p, v_win_p, k_win_s, v_win_s)
```

```python
from contextlib import ExitStack
import numpy as np
import ml_dtypes
import concourse.bass as bass
import concourse.mybir as mybir
from concourse.bass_utils import run_bass_kernel_spmd

F32 = mybir.dt.float32
BF16 = mybir.dt.bfloat16
ALU = mybir.AluOpType
AF = mybir.ActivationFunctionType

ENGS = ("pe", "act", "dve", "pool", "sp")
EPS = 1e-6
PAST = 16384
NS = 4


class Buf:
    __slots__ = ("name", "w", "r", "x")

    def __init__(self, name, x=False):
        self.name = name
        self.w = None
        self.r = {}
        self.x = x


class Lane:
    __slots__ = ("key", "sem", "val")

    def __init__(self, key, sem):
        self.key = key
        self.sem = sem
        self.val = 0


class Prog:
    def __init__(self, nc, stack):
        self.nc = nc
        self.stack = stack
        self.ops = {e: [] for e in ENGS}
        self.cnt = {e: 0 for e in ENGS}
        self.sems = {}
        for e in ENGS:
            self.sems[e] = stack.enter_context(nc.semaphore("s_" + e))
        self.seen = {e: {} for e in ENGS}
        self.nlanes = 0

    def lane(self):
        key = "L%d" % self.nlanes
        self.nlanes += 1
        sem = self.stack.enter_context(self.nc.semaphore(key))
        self.sems[key] = sem
        return Lane(key, sem)

    def _need(self, eng, reads, writes):
        need = {}
        for b in reads:
            if b.w is not None and need.get(b.w[0], 0) < b.w[1]:
                need[b.w[0]] = b.w[1]
            if b.x:
                for k, v in b.r.items():
                    if k != eng and need.get(k, 0) < v:
                        need[k] = v
        for b in writes:
            if b.w is not None and need.get(b.w[0], 0) < b.w[1]:
                need[b.w[0]] = b.w[1]
            for k, v in b.r.items():
                if need.get(k, 0) < v:
                    need[k] = v
        seen = self.seen[eng]
        waits = []
        for k, v in need.items():
            if k == "pe" and eng == "pe":
                continue
            if seen.get(k, 0) < v:
                seen[k] = v
                waits.append((k, v))
        return waits

    def _mark(self, tok, reads, writes):
        for b in reads:
            if b.r.get(tok[0], 0) < tok[1]:
                b.r[tok[0]] = tok[1]
        for b in writes:
            b.w = tok
            b.r = {}

    def op(self, eng, fn, reads=(), writes=(), signal=True):
        waits = self._need(eng, reads, writes)
        if signal:
            self.cnt[eng] += 1
            tok = (eng, self.cnt[eng])
        else:
            tok = (eng, self.cnt[eng] + 1)
        self._mark(tok, reads, writes)
        self.ops[eng].append((waits, fn, 1 if signal else 0, None))
        return tok

    def dma(self, q, out, in_, lane, reads=(), writes=(), **kw):
        waits = self._need(q, reads, writes)
        lane.val += 16
        tok = (lane.key, lane.val)
        self._mark(tok, reads, writes)

        def fn(e, out=out, in_=in_, kw=kw):
            return e.dma_start(out=out, in_=in_, **kw)

        self.ops[q].append((waits, fn, 0, lane))
        return tok

    def wait_tokens(self, eng, toks):
        waits = []
        for k, v in toks:
            if self.seen[eng].get(k, 0) < v:
                self.seen[eng][k] = v
                waits.append((k, v))
        self.ops[eng].append((waits, None, 0, None))

    def emit(self, block):
        sems = self.sems

        def run(ename, e):
            own = sems[ename]
            for waits, fn, sig, lane in self.ops[ename]:
                for k, v in waits:
                    e.wait_ge(sems[k], v)
                if fn is None:
                    continue
                ins = fn(e)
                if lane is not None:
                    ins.then_inc(lane.sem, 16)
                elif sig:
                    ins.then_inc(own, 1)

        @block.tensor
        def _(e):
            run("pe", e)

        @block.scalar
        def _(e):
            run("act", e)

        @block.vector
        def _(e):
            run("dve", e)

        @block.gpsimd
        def _(e):
            run("pool", e)

        @block.sync
        def _(e):
            run("sp", e)


def _gdec():
    return [1.0 - 2.0 ** (-5.0 - h) for h in range(4)]


def host_tables(SEQ, NSB):
    t = {}
    NTPS = SEQ // 512
    inv = (1.0 / (np.float32(10000.0) ** np.linspace(0.0, 1.0, 128, dtype=np.float32))).astype(np.float32)
    pos = np.arange(SEQ, dtype=np.float32)
    ang = (inv[:, None] * pos[None, :]).astype(np.float32)
    cs = np.stack([np.cos(ang), np.sin(ang)], axis=1).astype(np.float32)
    t["rope_r"] = np.ascontiguousarray(cs.reshape(128, 2, NTPS, 512).transpose(2, 0, 1, 3))
    pos_s = (np.float32(PAST) + np.arange(4, dtype=np.float32)).astype(np.float32)
    ang_s = (inv[:, None] * pos_s[None, :]).astype(np.float32)
    cs_s = np.stack([np.cos(ang_s), np.sin(ang_s)], axis=1).astype(np.float32)
    t["rope_rs"] = np.ascontiguousarray(np.tile(cs_s, (1, 1, NSB)))
    g = _gdec()
    i128 = np.arange(128, dtype=np.float64)
    dq = np.stack([np.power(g[h], i128 + 1) for h in range(4)], 0)
    t["dq"] = np.ascontiguousarray(np.broadcast_to(dq[None], (128, 4, 128))).astype(np.float32)
    mk = np.zeros((128, 4, 128), np.float64)
    for h in range(4):
        mk[:, h, :] = (i128[:, None] <= i128[None, :]) * np.power(g[h], -(i128[:, None] + 1)) / 16.0
    t["maskr"] = mk.astype(np.float32)
    t["kdec"] = np.stack([np.power(g[h], 127 - i128) / 16.0 for h in range(4)], 1).astype(np.float32)
    n = 4 * NSB
    tt = np.arange(n) % 4
    bb = np.arange(n) // 4
    dqs = np.stack([np.power(g[h], tt + 1.0) for h in range(4)], 0)
    t["dqs"] = np.ascontiguousarray(np.broadcast_to(dqs[None], (128, 4, n))).astype(np.float32)
    mks = np.zeros((n, 4, n), np.float64)
    for h in range(4):
        mks[:, h, :] = ((bb[:, None] == bb[None, :]) & (tt[:, None] <= tt[None, :])) * \
            np.power(g[h], -(tt[:, None] + 1.0)) / 16.0
    t["maskrs"] = mks.astype(np.float32)
    t["kdecs"] = np.stack([np.power(g[h], 3.0 - tt) / 16.0 for h in range(4)], 1).astype(np.float32)
    bm = (bb[:, None] == np.arange(NSB)[None, :]).astype(np.float32)
    t["bmrow"] = bm
    bmq = np.zeros((128, NSB, n), np.float32)
    bmq[:, bb, np.arange(n)] = 1.0
    t["bmq"] = bmq.astype(ml_dtypes.bfloat16)
    inv8 = (np.float32(500000.0) ** (-np.arange(8, dtype=np.float32) / np.float32(8.0))).astype(np.float32)
    anga = (pos[:, None] * inv8[None, :]).astype(np.float32)
    ca = np.stack([np.cos(anga), np.sin(anga)], 1).astype(np.float32)
    t["rope_a"] = np.ascontiguousarray(ca.reshape(SEQ // 128, 128, 2, 8).transpose(1, 0, 2, 3))
    angas = (pos_s[:, None] * inv8[None, :]).astype(np.float32)
    cas = np.stack([np.cos(angas), np.sin(angas)], 1).astype(np.float32)
    t["rope_as"] = np.ascontiguousarray(np.tile(cas, (NSB, 1, 1)))
    NEG = -30000.0
    kk = np.arange(128)
    prev = np.where(kk[:, None] > kk[None, :], 0.0, NEG)
    cur = np.where(kk[:, None] <= kk[None, :], 0.0, NEG)
    am = np.stack([np.tile(prev, (1, 4)), np.tile(cur, (1, 4))], 0)
    t["amask"] = am.astype(ml_dtypes.bfloat16)
    amc = np.full((NSB, 128, n), NEG)
    for b in range(NSB):
        for tq in range(4):
            amc[b, :, 4 * b + tq] = np.where(kk > tq, 0.0, NEG)
    t["amask_c"] = np.ascontiguousarray(np.tile(amc, (1, 1, 4))).astype(ml_dtypes.bfloat16)
    amn = np.where((bb[:, None] == bb[None, :]) & (tt[:, None] <= tt[None, :]), 0.0, NEG)
    t["amask_n"] = np.ascontiguousarray(np.tile(amn, (1, 4))).astype(ml_dtypes.bfloat16)
    return t


TABLE_DT = {"bmq": BF16, "amask": BF16, "amask_c": BF16, "amask_n": BF16}


def build(NSEQ, SEQ, NSB, tabs, do_sample=True):
    NTPS = SEQ // 512
    NSTOK = 4 * NSB
    nc = bass.Bass("TRN2", target_bir_lowering=False)

    def din(name, shape, dt=F32):
        return nc.dram_tensor(name, list(shape), dt, kind="ExternalInput").ap()

    def dout(name, shape):
        return nc.dram_tensor(name, list(shape), F32, kind="ExternalOutput").ap()

    xp = din("xp", [NSEQ * SEQ, 1024])
    xs = din("xs", [NSTOK, 1024])
    st_in = din("st_in", [NSB, 4, 256, 512])
    ck_in = din("ck_in", [NSB, 128, 256])
    cv_in = din("cv_in", [NSB, 128, 256])
    w_in = din("w_in", [1024, 6144])
    w_out = din("w_out", [2048, 1024])
    w_kv = din("w_kv", [1024, 512])
    w_q = din("w_q", [1024, 1024])
    w_o = din("w_o", [1024, 1024])
    w1 = din("w1", [2, 1024, 4096])
    w2 = din("w2", [2, 4096, 1024])
    gcol = din("gcol", [6, 1024])
    grow = din("grow", [4, 128, 1024])
    sinks = din("sinks", [128, 16])
    T = {k: din("t_" + k, v.shape, TABLE_DT.get(k, F32)) for k, v in tabs.items()}

    yp = dout("yp", [NSEQ * SEQ, 1024])
    ys = dout("ys", [NSTOK, 1024])
    sp_o = dout("sp_o", [NSEQ, 4, 256, 512])
    ss_o = dout("ss_o", [NSB, 4, 256, 512])
    kwp = dout("kwp", [NSEQ, 128, 256])
    vwp = dout("vwp", [NSEQ, 128, 256])
    kws = dout("kws", [NSB, 128, 256])
    vws = dout("vws", [NSB, 128, 256])

    slabs = []

    def add(W, r0, c0):
        slabs.append(W[r0:r0 + 1024, c0:c0 + 512].rearrange("(k p) n -> p k n", p=128))

    for s in range(12):
        add(w_in, 0, 512 * s)
    for ch in range(2):
        for kh in range(2):
            add(w_out, 1024 * kh, 512 * ch)

    def add_ffn(l):
        for s in range(8):
            add(w1[l], 0, 512 * s)
        for ch in range(2):
            for kq in range(4):
                add(w2[l], 1024 * kq, 512 * ch)

    add_ffn(0)
    add(w_kv, 0, 0)
    for ch in range(2):
        add(w_q, 0, 512 * ch)
    for ch in range(2):
        add(w_o, 0, 512 * ch)
    add_ffn(1)
    NSL = len(slabs)
    scr = nc.dram_tensor("wscr", [NSL, 128, 4096], BF16, kind="ExternalOutput").ap()


    n_tiles = NSEQ * NTPS + (1 if do_sample else 0)
    total_slabs = n_tiles * NSL

    with ExitStack() as st:
        P = Prog(nc, st)

        def sb(name, shape, dt):
            return st.enter_context(nc.sbuf_tensor(name, list(shape), dt))

        bufs = {}

        def B(*key):
            b = bufs.get(key)
            if b is None:
                b = bufs[key] = Buf(str(key), x=(key[0] == "ps"))
            return b

        hb = sb("h", [128, 4, 1024], F32)
        xTa = sb("xTa", [128, 8, 512], BF16)
        xTb = sb("xTb", [128, 8, 512], BF16)
        QKO = sb("QKO", [128, 16, 512], BF16)
        RA = sb("RA", [128, 16384], BF16)
        khat = sb("khat", [128, 2, 1024], BF16)
        Sf = sb("Sf", [128, 8, 512], F32)
        Sb = sb("Sb", [128, 8, 512], BF16)
        og = sb("og", [128, 2048], BF16)
        ring = sb("ring", [128, NS, 4096], BF16)
        ropeR = sb("ropeR", [128, 2, 2, 512], F32)
        gbuf = sb("gbuf", [128, 1024], F32)
        xn = sb("xn", [128, 2, 1024], BF16)
        junk = sb("junk", [128, 1024], BF16)
        rtmp = sb("rtmp", [128, 4, 512], F32)
        scb = sb("scb", [128, 2, 128], BF16)
        ident = sb("ident", [128, 128], BF16)
        identf = sb("identf", [128, 128], F32)
        dq_t = sb("dq_t", [128, 4, 128], F32)
        maskr_t = sb("maskr_t", [128, 4, 128], F32)
        kdec_t = sb("kdec_t", [128, 4], F32)
        amask_t = sb("amask_t", [128, 2, 512], BF16)
        gcol_t = sb("gcol_t", [128, 6, 8], F32)
        sexp_t = sb("sexp_t", [128, 16], F32)
        eps_t = sb("eps_t", [128, 1], F32)
        stat = sb("stat", [128, 64], F32)
        ropeA = sb("ropeA", [128, SEQ // 128, 2, 8], F32)
        ropeAs = sb("ropeAs", [128, 2, 8], F32)
        kf2 = sb("kf2", [128, 2, 256], F32)
        vf2 = sb("vf2", [128, 2, 256], F32)
        kb2 = sb("kb2", [128, 2, 256], BF16)
        kTs = sb("kTs", [64, 4, 5 * 128], BF16)
        vaug = sb("vaug", [128, 5, 4, 65], BF16)
        qf = sb("qf", [128, 1024], F32)
        qb = sb("qb", [128, 1024], BF16)
        atmp = sb("atmp", [128, 4, 16, 8], F32)
        ob = sb("ob", [128, 1024], BF16)
        rl = sb("rl", [128, 2, 512], F32)
        bmq_t = sb("bmq_t", [128, NSB, NSTOK], BF16)
        ps = st.enter_context(nc.psum_tensor("ps", [128, 8, 512], F32))
        block = st.enter_context(nc.Block())

        lanes_ring = [P.lane() for _ in range(NS)]
        lanes_scr = [P.lane() for _ in range(NS)]
        lane_h = [P.lane() for _ in range(4)]
        lane_y = [P.lane() for _ in range(4)]
        lane_c = P.lane()
        lane_rope = [P.lane() for _ in range(2)]
        lane_g = P.lane()
        lane_so = P.lane()
        lane_kv = P.lane()
        lane_vv = P.lane()
        out_tokens = []

        pstate = {}

        def nb(n=1, lo=0, hi=8):
            bi = pstate.get((lo, hi), 0)
            if n > 1 and bi % n:
                bi += n - bi % n
            w = hi - lo
            r = [lo + (bi + i) % w for i in range(n)]
            pstate[(lo, hi)] = (bi + n) % w
            return r

        def PB(i):
            return B("ps", i)

        import os
        KSKIP = os.environ.get("KSKIP", "")
        wst = {"next": 0, "issued": 0}

        def w_issue(g):
            if g >= total_slabs:
                return
            t_, i = divmod(g, NSL)
            slot = g % NS
            dst = ring[:, slot, :]
            if t_ == 0:
                P.dma("pool", dst.rearrange("p (k n) -> p k n", k=8), slabs[i], lanes_ring[slot], writes=[B("ring", slot)])
                P.dma("sp", scr[i], dst, lanes_scr[slot], reads=[B("ring", slot)], writes=[B("scr", i)])
            else:
                P.dma("sp", dst, scr[i], lanes_ring[slot], reads=[B("scr", i)], writes=[B("ring", slot)])

        def w_get():
            g = wst["next"]
            wst["next"] += 1
            slot = g % NS
            return ring[:, slot, :].rearrange("p (k n) -> p k n", k=8), B("ring", slot), g

        def w_done(g):
            w_issue(g + NS)

        import os
        KSKIP = os.environ.get("KSKIP", "")
        if "w" not in KSKIP:
            for g in range(NS):
                w_issue(g)

        if "c" not in KSKIP:
          P.dma("sp", dq_t[:], T["dq"], lane_c, writes=[B("c")])
        if "c" not in KSKIP:
          P.dma("sp", maskr_t[:], T["maskr"], lane_c, writes=[B("c")])
        if "c" not in KSKIP:
          P.dma("sp", kdec_t[:], T["kdec"], lane_c, writes=[B("c")])
        if "c" not in KSKIP:
          P.dma("sp", amask_t[:], T["amask"].rearrange("b k q -> k b q"), lane_c, writes=[B("c")])
        if "g" not in KSKIP:
          P.dma("sp", gcol_t[:], gcol.rearrange("g (k p) -> p g k", p=128), lane_c, writes=[B("c")],
              allow_slow_non_contiguous=True)
        if "c" not in KSKIP:
          P.dma("sp", sexp_t[:], sinks, lane_c, writes=[B("c")])
        if "c" not in KSKIP:
          P.dma("sp", ropeA[:], T["rope_a"], lane_c, writes=[B("c")])
        P.op("pool", lambda e: e.memset(identf[:], 0.0), writes=[B("identf")])
        P.op("pool", lambda e: e.affine_select(out=identf[:], in_=identf[:], compare_op=ALU.not_equal, fill=1.0,
                                               base=0, pattern=[[-1, 128]], channel_multiplier=1),
             reads=[B("identf")], writes=[B("identf")])
        P.op("pool", lambda e: e.tensor_copy(ident[:], identf[:]), reads=[B("identf")], writes=[B("c2")])
        P.op("pool", lambda e: e.memset(eps_t[:], EPS), writes=[B("c2")])
        if "v" not in KSKIP:
          P.op("pool", lambda e: e.memset(vaug[:].rearrange("p a b c -> p (a b c)"), 1.0), writes=[B("vaug", i) for i in range(5)])
        P.op("act", lambda e: e.activation(out=sexp_t[:], in_=sexp_t[:], func=AF.Exp), reads=[B("c")], writes=[B("c")])
        CB = [B("c"), B("c2")]
        gdec = _gdec()

        def rstd_from_ms(ms_ap, out_ap, n, rds, wr):
            P.op("act", lambda e: e.activation(out=out_ap, in_=ms_ap, func=AF.Sqrt, bias=eps_t[0:n, 0:1], scale=1.0),
                 reads=rds + CB, writes=[wr])
            P.op("dve", lambda e: e.reciprocal(out_ap, out_ap), reads=[wr], writes=[wr])

        def norm_phase(nt, nch, dests):
            for c in range(nch):
                ms = stat[0:nt, c:c + 1]
                P.op("act", lambda e, c=c, ms=ms: e.activation(out=junk[0:nt, :], in_=hb[0:nt, c, :], func=AF.Square,
                                                               scale=1.0 / 32, accum_out=ms),
                     reads=[B("h", c)], writes=[B("junk"), B("ms", c)])
            rstd_from_ms(stat[0:nt, 0:nch], stat[0:nt, 4:4 + nch], nt, [B("ms", c) for c in range(nch)], B("rsall"))
            for c in range(nch):
                rs = stat[0:nt, 4 + c:5 + c]
                xi = c % 2
                if c % 2 == 0:
                    P.op("act", lambda e, c=c, rs=rs, xi=xi: e.activation(out=xn[0:nt, xi, :], in_=hb[0:nt, c, :], func=AF.Copy, scale=rs),
                         reads=[B("h", c), B("rsall")], writes=[B("xn", xi)])
                else:
                    P.op("dve", lambda e, c=c, rs=rs, xi=xi: e.tensor_scalar(xn[0:nt, xi, :], hb[0:nt, c, :], rs, None, op0=ALU.mult),
                         reads=[B("h", c), B("rsall")], writes=[B("xn", xi)])
                (bk,) = nb()
                psb = ps[:, bk, :].bitcast(BF16)
                for k in range(8):
                    P.op("pe", lambda e, k=k, xi=xi, psb=psb: e.transpose(psb[:, k * nt:(k + 1) * nt], xn[0:nt, xi, k * 128:(k + 1) * 128], ident[0:nt, 0:nt]),
                         reads=[B("xn", xi)] + CB, writes=[PB(bk)], signal=(k == 7))
                for gi, dst, nm in dests:
                    P.op("dve", lambda e, gi=gi, dst=dst, c=c, psb=psb: e.tensor_tensor(
                        out=dst[:, :, c * nt:(c + 1) * nt], in0=psb[:, 0:8 * nt].rearrange("p (k t) -> p k t", k=8),
                        in1=gcol_t[:, gi, :].unsqueeze(2).to_broadcast([128, 8, nt]), op=ALU.mult),
                         reads=[PB(bk)] + CB, writes=[B(nm, c)])

        def load_g(gi):
            P.dma("sp", gbuf[:], grow[gi], lane_g, writes=[B("gbuf")])

        def ffn_phase(nt, nch, l, gi_pre, gi_post, after_chunk=None):
            TT = nt * nch
            norm_phase(nt, nch, [(gi_pre, xTa, "xTa")])
            uT = RA[:].rearrange("p (j t) -> p j t", j=32)
            ri = 0
            for s in range(8):
                wt, wb, g = w_get()
                for j in range(4):
                    (bk,) = nb()
                    for k in range(8):
                        P.op("pe", lambda e, k=k, j=j, wt=wt, bk=bk: e.matmul(
                            ps[:, bk, 0:TT], lhsT=wt[:, k, j * 128:(j + 1) * 128], rhs=xTa[:, k, 0:TT],
                            start=(k == 0), stop=(k == 7)),
                             reads=[B("xTa", c) for c in range(nch)] + [wb], writes=[PB(bk)], signal=(k == 7))
                    fj = 4 * s + j
                    r = ri % 2
                    ri += 1
                    P.op("act", lambda e, bk=bk, r=r: e.activation(out=rl[:, r, 0:TT], in_=ps[:, bk, 0:TT], func=AF.Relu),
                         reads=[PB(bk)], writes=[B("rl", r)])
                    P.op("pool", lambda e, fj=fj, r=r: e.tensor_tensor(out=uT[:, fj, 0:TT], in0=rl[:, r, 0:TT], in1=rl[:, r, 0:TT], op=ALU.mult),
                         reads=[B("rl", r)], writes=[B("RA", fj)])
                w_done(g)
            dense_out_phase_named(nt, nch, uT, lambda fk, c: B("RA", fk), 4, gi_post, after_chunk)

        def dense_out_phase_named(nt, nch, srcT, bufof, nkg, gi_post, after_chunk=None):
            load_g(gi_post)
            bank_of = {}
            for ch in range(2):
                bks = nb(4) if nch > 1 else nb(1)
                for kg in range(nkg):
                    wt, wb, g = w_get()
                    for c in range(nch):
                        for k in range(8):
                            fk = kg * 8 + k
                            P.op("pe", lambda e, c=c, k=k, fk=fk, wt=wt, bk=bks[c], kg=kg: e.matmul(
                                ps[0:nt, bk, :], lhsT=srcT[:, fk, c * nt:(c + 1) * nt], rhs=wt[:, k, :],
                                start=(kg == 0 and k == 0), stop=(kg == nkg - 1 and k == 7)),
                                 reads=[bufof(fk, c), wb], writes=[PB(bks[c])],
                                 signal=(k == 7))
                    w_done(g)
                for c in range(nch):
                    bank_of[(c, ch)] = bks[c]
                    ms = stat[0:nt, 8 + 2 * c + ch:9 + 2 * c + ch]
                    P.op("act", lambda e, c=c, ms=ms, bk=bks[c]: e.activation(out=junk[0:nt, 0:512], in_=ps[0:nt, bk, :], func=AF.Square,
                                                                             scale=1.0 / 32, accum_out=ms),
                         reads=[PB(bks[c])], writes=[B("junk"), B("ms2", c, ch)])
                    if ch == 0:
                        P.op("dve", lambda e, c=c, bk=bks[c]: e.tensor_tensor(out=rtmp[0:nt, c, :], in0=ps[0:nt, bk, :],
                                                                            in1=gbuf[0:nt, 0:512], op=ALU.mult),
                             reads=[PB(bks[c]), B("gbuf")], writes=[B("rt", c)])
            for c in range(nch):
                P.op("dve", lambda e, c=c: e.tensor_tensor(out=stat[0:nt, 16 + c:17 + c], in0=stat[0:nt, 8 + 2 * c:9 + 2 * c],
                                                           in1=stat[0:nt, 9 + 2 * c:10 + 2 * c], op=ALU.add),
                     reads=[B("ms2", c, 0), B("ms2", c, 1)], writes=[B("ms3", c)])
            rstd_from_ms(stat[0:nt, 16:16 + nch], stat[0:nt, 20:20 + nch], nt, [B("ms3", c) for c in range(nch)], B("rs3"))
            for c in range(nch):
                rs = stat[0:nt, 20 + c:21 + c]
                P.op("dve", lambda e, c=c, rs=rs: e.scalar_tensor_tensor(
                    out=hb[0:nt, c, 0:512], in0=rtmp[0:nt, c, :], scalar=rs, in1=hb[0:nt, c, 0:512],
                    op0=ALU.mult, op1=ALU.add),
                     reads=[B("rt", c), B("rs3"), B("h", c)], writes=[B("h", c)])
                bk = bank_of[(c, 1)]
                r2 = c % 2
                tmp = rl[0:nt, r2, :]
                P.op("dve", lambda e, bk=bk, tmp=tmp: e.tensor_tensor(out=tmp, in0=ps[0:nt, bk, :],
                                                                    in1=gbuf[0:nt, 512:1024], op=ALU.mult),
                     reads=[PB(bk), B("gbuf")], writes=[B("rl", r2)])
                P.op("dve", lambda e, c=c, tmp=tmp, rs=rs: e.scalar_tensor_tensor(
                    out=hb[0:nt, c, 512:1024], in0=tmp, scalar=rs, in1=hb[0:nt, c, 512:1024],
                    op0=ALU.mult, op1=ALU.add),
                     reads=[B("rl", r2), B("rs3"), B("h", c)], writes=[B("h", c)])
                if after_chunk is not None:
                    after_chunk(c)

        def ret_layer(nt, nch, sample, seq_first, seq_last, seq_idx, ropeT, dqT, maskT, kdecT):
            TT = nt * nch
            norm_phase(nt, nch, [(0, xTa, "xTa")])
            xa_r = [B("xTa", c) for c in range(nch)]
            import os
            RSTOP = int(os.environ.get("RSTOP", "99"))
            if RSTOP < 1:
                return
            for s in range(4):
                wt, wb, g = w_get()
                bks = nb(4)
                for j in range(4):
                    for k in range(8):
                        P.op("pe", lambda e, k=k, j=j, wt=wt, bk=bks[j]: e.matmul(
                            ps[:, bk, 0:TT], lhsT=wt[:, k, j * 128:(j + 1) * 128], rhs=xTa[:, k, 0:TT],
                            start=(k == 0), stop=(k == 7)),
                             reads=xa_r + [wb], writes=[PB(bks[j])], signal=(k == 7))
                w_done(g)
                for hh in range(2):
                    b1, b2 = bks[2 * hh], bks[2 * hh + 1]
                    f1 = 4 * s + 2 * hh
                    head = (f1 % 8) // 2
                    isq = s < 2
                    X1, X2 = ps[:, b1, 0:TT], ps[:, b2, 0:TT]
                    C_, S_ = ropeT[:, 0, 0:TT], ropeT[:, 1, 0:TT]
                    t = [rtmp[:, i, 0:TT] for i in range(4)]
                    P.op("dve", lambda e, X1=X1, C_=C_, t=t: e.tensor_tensor(out=t[0], in0=X1, in1=C_, op=ALU.mult),
                         reads=[PB(b1), B("rope")], writes=[B("rt", 0)])
                    P.op("dve", lambda e, X2=X2, S_=S_, t=t: e.tensor_tensor(out=t[1], in0=X2, in1=S_, op=ALU.mult),
                         reads=[PB(b2), B("rope")], writes=[B("rt", 1)])
                    P.op("dve", lambda e, X2=X2, C_=C_, t=t: e.tensor_tensor(out=t[2], in0=X2, in1=C_, op=ALU.mult),
                         reads=[PB(b2), B("rope")], writes=[B("rt", 2)])
                    P.op("dve", lambda e, X1=X1, S_=S_, t=t: e.tensor_tensor(out=t[3], in0=X1, in1=S_, op=ALU.mult),
                         reads=[PB(b1), B("rope")], writes=[B("rt", 3)])
                    w1b = [B("QKO", f1, c) for c in range(nch)]
                    w2b = [B("QKO", f1 + 1, c) for c in range(nch)]
                    if isq:
                        dqv = dqT[:, head, :].unsqueeze(1).to_broadcast([128, nch, nt])
                        P.op("pool", lambda e, t=t: e.tensor_tensor(out=t[0], in0=t[0], in1=t[1], op=ALU.subtract),
                             reads=[B("rt", 0), B("rt", 1)], writes=[B("rt", 0)])
                        P.op("pool", lambda e, t=t, f1=f1, dqv=dqv: e.tensor_tensor(
                            out=QKO[:, f1, 0:TT].rearrange("p (c t) -> p c t", c=nch), in0=t[0].rearrange("p (c t) -> p c t", c=nch),
                            in1=dqv, op=ALU.mult), reads=[B("rt", 0)] + CB, writes=w1b)
                        P.op("pool", lambda e, t=t: e.tensor_tensor(out=t[2], in0=t[2], in1=t[3], op=ALU.add),
                             reads=[B("rt", 2), B("rt", 3)], writes=[B("rt", 2)])
                        P.op("pool", lambda e, t=t, f1=f1, dqv=dqv: e.tensor_tensor(
                            out=QKO[:, f1 + 1, 0:TT].rearrange("p (c t) -> p c t", c=nch), in0=t[2].rearrange("p (c t) -> p c t", c=nch),
                            in1=dqv, op=ALU.mult), reads=[B("rt", 2)] + CB, writes=w2b)
                    else:
                        P.op("pool", lambda e, t=t, f1=f1: e.tensor_tensor(out=QKO[:, f1, 0:TT], in0=t[0], in1=t[1], op=ALU.subtract),
                             reads=[B("rt", 0), B("rt", 1)], writes=w1b)
                        P.op("pool", lambda e, t=t, f1=f1: e.tensor_tensor(out=QKO[:, f1 + 1, 0:TT], in0=t[2], in1=t[3], op=ALU.add),
                             reads=[B("rt", 2), B("rt", 3)], writes=w2b)
            if RSTOP < 2:
                return
            for s in range(8):
                wt, wb, g = w_get()
                hv = s % 4
                for c in range(nch):
                    (bk,) = nb()
                    for k in range(8):
                        P.op("pe", lambda e, k=k, c=c, wt=wt, bk=bk: e.matmul(
                            ps[0:nt, bk, :], lhsT=xTa[:, k, c * nt:(c + 1) * nt], rhs=wt[:, k, :],
                            start=(k == 0), stop=(k == 7)),
                             reads=[B("xTa", c), wb], writes=[PB(bk)], signal=(k == 7))
                    if s < 4:
                        u = 4 * c + hv
                        P.op("act", lambda e, bk=bk, u=u: e.activation(out=RA[0:nt, u * 512:(u + 1) * 512], in_=ps[0:nt, bk, :], func=AF.Copy),
                             reads=[PB(bk)], writes=[B("RA", u)])
                    else:
                        u = 16 + 4 * c + hv
                        P.op("act", lambda e, bk=bk, u=u: e.activation(out=RA[0:nt, u * 512:(u + 1) * 512], in_=ps[0:nt, bk, :], func=AF.Silu),
                             reads=[PB(bk)], writes=[B("RA", u)])
                w_done(g)
            if RSTOP < 3:
                return
            for c in range(nch):
                first = seq_first and c == 0
                last = seq_last and c == nch - 1
                cs = slice(c * nt, (c + 1) * nt)
                ki = c % 2
                if True:
                    (bk,) = nb(1, 4, 8)
                    psb = ps[:, bk, :].bitcast(BF16)
                    for kc in range(8):
                        P.op("pe", lambda e, kc=kc, psb=psb, cs=cs: e.transpose(psb[0:nt, kc * 128:(kc + 1) * 128], QKO[:, 8 + kc, cs], ident[:]),
                             reads=[B("QKO", 8 + kc, c)] + CB, writes=[PB(bk)], signal=(kc == 7))
                    for h in range(4):
                        P.op("dve", lambda e, h=h, psb=psb, ki=ki: e.tensor_scalar(
                            khat[0:nt, ki, h * 256:(h + 1) * 256], psb[0:nt, h * 256:(h + 1) * 256], kdecT[0:nt, h:h + 1], None, op0=ALU.mult),
                             reads=[PB(bk)] + CB, writes=[B("khat", ki, h)])
                obk = [0, 1, 2, 3]
                for h in range(4):
                    (bk,) = nb(1, 4, 8)
                    for kk in range(2):
                        P.op("pe", lambda e, h=h, kk=kk, bk=bk, cs=cs: e.matmul(
                            ps[0:nt, bk, 0:nt], lhsT=QKO[:, 8 + 2 * h + kk, cs], rhs=QKO[:, 2 * h + kk, cs],
                            start=(kk == 0), stop=(kk == 1)),
                             reads=[B("QKO", 8 + 2 * h + kk, c), B("QKO", 2 * h + kk, c)], writes=[PB(bk)], signal=(kk == 1))
                    si = h % 2
                    P.op("dve", lambda e, h=h, bk=bk, si=si: e.tensor_tensor(out=scb[0:nt, si, 0:nt], in0=ps[0:nt, bk, 0:nt],
                                                                           in1=maskT[0:nt, h, 0:nt], op=ALU.mult),
                         reads=[PB(bk)] + CB, writes=[B("scb", si)])
                    vu = 4 * c + h
                    ob_ = obk[h]
                    cross = (not first) and (not sample)
                    P.op("pe", lambda e, si=si, vu=vu, ob_=ob_, cross=cross: e.matmul(
                        ps[0:nt, ob_, :], lhsT=scb[0:nt, si, 0:nt], rhs=RA[0:nt, vu * 512:(vu + 1) * 512], start=True,
                        stop=(not cross and not sample)),
                         reads=[B("scb", si), B("RA", vu)], writes=[PB(ob_)], signal=(not cross))
                    if cross:
                        for kk in range(2):
                            P.op("pe", lambda e, h=h, kk=kk, ob_=ob_, cs=cs: e.matmul(
                                ps[0:nt, ob_, :], lhsT=QKO[:, 2 * h + kk, cs], rhs=Sb[:, 2 * h + kk, :], start=False, stop=(kk == 1)),
                                 reads=[B("QKO", 2 * h + kk, c), B("Sb", 2 * h + kk)], writes=[PB(ob_)], signal=(kk == 1))
                    ms = stat[0:nt, 24 + h:25 + h]
                    if not sample:
                        P.op("act", lambda e, ob_=ob_, ms=ms: e.activation(out=junk[0:nt, 0:512], in_=ps[0:nt, ob_, :], func=AF.Square,
                                                                            scale=float(512 ** -0.5), accum_out=ms),
                             reads=[PB(ob_)], writes=[B("junk"), B("mso", h)])
                    if not sample:
                        for kk in range(2):
                            (sbk,) = nb(1, 4, 8)
                            P.op("pe", lambda e, h=h, kk=kk, sbk=sbk, ki=ki, vu=vu: e.matmul(
                                ps[:, sbk, :], lhsT=khat[0:nt, ki, h * 256 + kk * 128:h * 256 + (kk + 1) * 128],
                                rhs=RA[0:nt, vu * 512:(vu + 1) * 512], start=True, stop=True),
                                 reads=[B("khat", ki, h), B("RA", vu)], writes=[PB(sbk)])
                            si_ = 2 * h + kk
                            if first:
                                P.op("dve", lambda e, sbk=sbk, si_=si_: e.tensor_copy(Sf[:, si_, :], ps[:, sbk, :]),
                                     reads=[PB(sbk)], writes=[B("Sf", si_)])
                            else:
                                P.op("dve", lambda e, sbk=sbk, si_=si_, h=h: e.scalar_tensor_tensor(
                                    out=Sf[:, si_, :], in0=Sf[:, si_, :], scalar=float(gdec[h] ** 128), in1=ps[:, sbk, :],
                                    op0=ALU.mult, op1=ALU.add),
                                     reads=[PB(sbk), B("Sf", si_)], writes=[B("Sf", si_)])
                            if not last:
                                P.op("act", lambda e, si_=si_: e.activation(out=Sb[:, si_, :], in_=Sf[:, si_, :], func=AF.Copy),
                                     reads=[B("Sf", si_)], writes=[B("Sb", si_)])
                if sample:
                    sample_cross(obk)
                    for h in range(4):
                        P.op("act", lambda e, h=h: e.activation(out=junk[0:nt, 0:512], in_=ps[0:nt, obk[h], :], func=AF.Square,
                                                                scale=float(512 ** -0.5), accum_out=stat[0:nt, 24 + h:25 + h]),
                             reads=[PB(obk[h])], writes=[B("junk"), B("mso", h)])
                rs4 = stat[0:nt, 28:32]
                rstd_from_ms(stat[0:nt, 24:28], rs4, nt, [B("mso", h) for h in range(4)], B("rso"))
                for h in range(4):
                    P.op("dve", lambda e, h=h, c=c: e.scalar_tensor_tensor(
                        out=og[0:nt, h * 512:(h + 1) * 512], in0=ps[0:nt, obk[h], :], scalar=stat[0:nt, 28 + h:29 + h],
                        in1=RA[0:nt, (16 + 4 * c + h) * 512:(17 + 4 * c + h) * 512], op0=ALU.mult, op1=ALU.mult),
                         reads=[PB(obk[h]), B("rso"), B("RA", 16 + 4 * c + h)], writes=[B("og", h)])
                if last and not sample:
                    tok = P.dma("sp", sp_o[seq_idx].rearrange("h (kk p) e -> p (h kk) e", p=128), Sf[:], lane_so,
                                reads=[B("Sf", i) for i in range(8)])
                    out_tokens.append(tok)
                tb = nb(2, 4, 8)
                for f in range(16):
                    bk = tb[f // 8]
                    psb = ps[:, bk, :].bitcast(BF16)
                    P.op("pe", lambda e, f=f, psb=psb: e.transpose(psb[:, (f % 8) * nt:(f % 8 + 1) * nt], og[0:nt, f * 128:(f + 1) * 128], ident[0:nt, 0:nt]),
                         reads=[B("og", f // 4)] + CB, writes=[PB(bk)], signal=(f % 8 == 7))
                for half in range(2):
                    bk = tb[half]
                    psb = ps[:, bk, :].bitcast(BF16)
                    P.op("act", lambda e, half=half, psb=psb, cs=cs: e.activation(
                        out=QKO[:, half * 8:(half + 1) * 8, cs], in_=psb[:, 0:8 * nt].rearrange("p (k t) -> p k t", k=8), func=AF.Copy),
                         reads=[PB(bk)], writes=[B("QKO", half * 8 + k, c) for k in range(8)])
            if os.environ.get("DBG"):
                ld = P.lane()
                out_tokens.append(P.dma("pool", ys[0:64, :], RA[0:64, 0:1024], ld, reads=[B("RA", 0), B("RA", 1)]))
                ld2 = P.lane()
                out_tokens.append(P.dma("pool", kws[0], khat[:, 1, 0:256], ld2, reads=[B("khat", 1, 0)]))
                if not seq_last:
                    ld3 = P.lane()
                    out_tokens.append(P.dma("sp", sp_o[seq_idx].rearrange("h (kk p) e -> p (h kk) e", p=128), Sf[:], ld3,
                                            reads=[B("Sf", i) for i in range(8)]))
            if RSTOP < 4:
                return
            dense_out_phase_named(nt, nch, QKO, lambda fk, c: B("QKO", fk, c), 2, 0)

        lane_sf = [P.lane(), P.lane()]
        lane_so2 = [P.lane(), P.lane()]

        def qx_base(h, kk):
            return (4 if h < 2 else 20) * 512 + ((h % 2) * 2 + kk) * 1024

        def sample_cross(obk):
            nt = NSTOK
            for h in range(4):
                for kk in range(2):
                    base = qx_base(h, kk)
                    u0 = base // 512
                    P.op("pool", lambda e, h=h, kk=kk, base=base: e.tensor_tensor(
                        out=RA[:, base:base + NSB * nt].rearrange("p (b t) -> p b t", b=NSB),
                        in0=QKO[:, 2 * h + kk, 0:nt].unsqueeze(1).to_broadcast([128, NSB, nt]), in1=bmq_t[:], op=ALU.mult),
                         reads=[B("QKO", 2 * h + kk, 0)] + CB, writes=[B("RA", u0), B("RA", u0 + 1)])
            for b in range(NSB):
                i2 = b % 2
                P.op("dve", lambda e, b=b, i2=i2: e.tensor_scalar(og[0:nt, i2 * 1024:(i2 + 1) * 1024], khat[0:nt, 0, :],
                                                                   stat[0:nt, 48 + b:49 + b], None, op0=ALU.mult),
                     reads=[B("khat", 0, h) for h in range(4)] + CB, writes=[B("og", 2 * i2), B("og", 2 * i2 + 1)])
                for hp in range(2):
                    sfb = [B("Sf", 4 * hp + i) for i in range(4)]
                    P.dma("sp", Sf[:, 4 * hp:4 * hp + 4, :], st_in[b, 2 * hp:2 * hp + 2].rearrange("h (kk p) e -> p (h kk) e", p=128),
                          lane_sf[hp], writes=sfb)
                    for i in range(4):
                        si_ = 4 * hp + i
                        if i % 2 == 0:
                            P.op("act", lambda e, si_=si_: e.activation(out=Sb[:, si_, :], in_=Sf[:, si_, :], func=AF.Copy),
                                 reads=[B("Sf", si_)], writes=[B("Sb", si_)])
                        else:
                            P.op("pool", lambda e, si_=si_: e.tensor_copy(Sb[:, si_, :], Sf[:, si_, :]),
                                 reads=[B("Sf", si_)], writes=[B("Sb", si_)])
                    for hh in range(2):
                        h = 2 * hp + hh
                        for kk in range(2):
                            si_ = 2 * h + kk
                            base = qx_base(h, kk) + b * nt
                            last = (b == NSB - 1 and kk == 1)
                            P.op("pe", lambda e, h=h, si_=si_, base=base, last=last: e.matmul(
                                ps[0:nt, obk[h], :], lhsT=RA[:, base:base + nt], rhs=Sb[:, si_, :], start=False, stop=last),
                                 reads=[B("RA", base // 512), B("Sb", si_)], writes=[PB(obk[h])])
                            (sbk,) = nb(1, 4, 8)
                            co = i2 * 1024 + h * 256 + kk * 128
                            P.op("pe", lambda e, h=h, sbk=sbk, co=co: e.matmul(
                                ps[:, sbk, :], lhsT=og[0:nt, co:co + 128], rhs=RA[0:nt, h * 512:(h + 1) * 512], start=True, stop=True),
                                 reads=[B("og", co // 512), B("RA", h)], writes=[PB(sbk)])
                            P.op("dve", lambda e, sbk=sbk, si_=si_, h=h: e.scalar_tensor_tensor(
                                out=Sf[:, si_, :], in0=Sf[:, si_, :], scalar=float(gdec[h] ** 4), in1=ps[:, sbk, :],
                                op0=ALU.mult, op1=ALU.add),
                                 reads=[PB(sbk), B("Sf", si_)], writes=[B("Sf", si_)])
                    out_tokens.append(P.dma("sp", ss_o[b, 2 * hp:2 * hp + 2].rearrange("h (kk p) e -> p (h kk) e", p=128),
                                            Sf[:, 4 * hp:4 * hp + 4, :], lane_so2[hp], reads=sfb))

        def swa_layer(nt, nch, sample, seq_first, seq_last, seq_idx, chunk0):
            TT = nt * nch
            import os
            norm_phase(nt, nch, [(2, xTa, "xTa"), (3, xTb, "xTb")])
            wt, wb, g = w_get()

            def kv_mm(c):
                (bk,) = nb()
                for k in range(8):
                    P.op("pe", lambda e, k=k, c=c, bk=bk, wt=wt: e.matmul(
                        ps[0:nt, bk, :], lhsT=xTa[:, k, c * nt:(c + 1) * nt], rhs=wt[:, k, :], start=(k == 0), stop=(k == 7)),
                         reads=[B("xTa", c), wb], writes=[PB(bk)], signal=(k == 7))
                return bk

            def kv_post(c, bk):
                slot = c + 1
                i2 = c % 2
                kf, vf, kb = kf2[:, i2, :], vf2[:, i2, :], kb2[:, i2, :]
                P.op("act", lambda e: e.activation(out=kf[0:nt, :], in_=ps[0:nt, bk, 0:256], func=AF.Copy),
                     reads=[PB(bk)], writes=[B("kf", i2)])
                P.op("act", lambda e: e.activation(out=vf[0:nt, :], in_=ps[0:nt, bk, 256:512], func=AF.Copy),
                     reads=[PB(bk)], writes=[B("vf", i2)])
                P.op("dve", lambda e: e.tensor_copy(vaug[0:nt, slot, :, 0:64], ps[0:nt, bk, 256:512].rearrange("p (h d) -> p h d", h=4)),
                     reads=[PB(bk)], writes=[B("vaug", slot)])
                if sample:
                    rc, rsn = ropeAs[0:nt, 0, :], ropeAs[0:nt, 1, :]
                else:
                    rc, rsn = ropeA[0:nt, chunk0 + c, 0, :], ropeA[0:nt, chunk0 + c, 1, :]
                rope_tok(kf, 4, nt, rc, rsn, ("kf", i2))
                P.op("pool", lambda e: e.tensor_copy(kb[0:nt, :], kf[0:nt, :]), reads=[B("kf", i2)], writes=[B("kb", i2)])
                (tbk,) = nb()
                psb = ps[:, tbk, :].bitcast(BF16)
                for kv in range(4):
                    P.op("pe", lambda e, kv=kv: e.transpose(psb[0:64, kv * nt:(kv + 1) * nt], kb[0:nt, kv * 64:(kv + 1) * 64], ident[0:nt, 0:nt]),
                         reads=[B("kb", i2)] + CB, writes=[PB(tbk)], signal=(kv == 3))
                P.op("dve", lambda e: e.tensor_copy(kTs[:, :, slot * 128:slot * 128 + nt],
                                                    psb[0:64, 0:4 * nt].rearrange("p (h t) -> p h t", h=4)),
                     reads=[PB(tbk)], writes=[B("kTs", slot)])
                if seq_last and c == nch - 1 and not sample:
                    out_tokens.append(P.dma("sp", kwp[seq_idx], kf[:, :], lane_kv, reads=[B("kf", i2)]))
                    out_tokens.append(P.dma("sp", vwp[seq_idx], vf[:, :], lane_vv, reads=[B("vf", i2)]))
                if sample:
                    for b in range(NSB):
                        out_tokens.append(P.dma("sp", kws[b, 124:128, :], kf[4 * b:4 * b + 4, :], lane_kv, reads=[B("kf", i2)]))
                        out_tokens.append(P.dma("sp", vws[b, 124:128, :], vf[4 * b:4 * b + 4, :], lane_vv, reads=[B("vf", i2)]))

            kvb = {0: kv_mm(0)}
            for c in range(1, nch):
                kvb[c] = kv_mm(c)
                kv_post(c - 1, kvb[c - 1])
            w_done(g)
            kv_post(nch - 1, kvb[nch - 1])
            import os
            SSTOP = int(os.environ.get("SSTOP", "99"))
            if SSTOP < 1:
                return
            qTs = QKO[0:64, :, :]

            def q_mm(ch, c, wt, wb):
                (bk,) = nb()
                for k in range(8):
                    P.op("pe", lambda e, k=k, c=c, wt=wt, bk=bk: e.matmul(
                        ps[0:nt, bk, :], lhsT=xTb[:, k, c * nt:(c + 1) * nt], rhs=wt[:, k, :], start=(k == 0), stop=(k == 7)),
                         reads=[B("xTb", c), wb], writes=[PB(bk)], signal=(k == 7))
                return bk

            def q_post(ch, c, bk, gi_):
                i2 = gi_ % 2
                qfv = qf[:, i2 * 512:(i2 + 1) * 512]
                qbv = qb[:, i2 * 512:(i2 + 1) * 512]
                P.op("act", lambda e: e.activation(out=qfv[0:nt, :], in_=ps[0:nt, bk, :], func=AF.Copy),
                     reads=[PB(bk)], writes=[B("qf", i2)])
                if sample:
                    rc, rsn = ropeAs[0:nt, 0, :], ropeAs[0:nt, 1, :]
                else:
                    rc, rsn = ropeA[0:nt, chunk0 + c, 0, :], ropeA[0:nt, chunk0 + c, 1, :]
                rope_tok(qfv, 8, nt, rc, rsn, ("qf", i2))
                P.op("pool", lambda e: e.tensor_copy(qbv[0:nt, :], qfv[0:nt, :]), reads=[B("qf", i2)], writes=[B("qb", i2)])
                (tbk,) = nb()
                psb = ps[:, tbk, :].bitcast(BF16)
                for hh in range(8):
                    P.op("pe", lambda e, hh=hh: e.transpose(psb[0:64, hh * nt:(hh + 1) * nt], qbv[0:nt, hh * 64:(hh + 1) * 64], ident[0:nt, 0:nt]),
                         reads=[B("qb", i2)] + CB, writes=[PB(tbk)], signal=(hh == 7))
                P.op("dve", lambda e: e.tensor_copy(
                    qTs[:, ch * 8:(ch + 1) * 8, c * nt:(c + 1) * nt], psb[0:64, 0:8 * nt].rearrange("p (h t) -> p h t", h=8)),
                     reads=[PB(tbk)], writes=[B("QKO", ch * 8 + hh, c) for hh in range(8)])

            pend = None
            gi_ = 0
            for ch in range(2):
                wt, wb, g = w_get()
                for c in range(nch):
                    bk = q_mm(ch, c, wt, wb)
                    if pend is not None:
                        q_post(*pend)
                    pend = (ch, c, bk, gi_)
                    gi_ += 1
                w_done(g)
            q_post(*pend)
            if SSTOP < 2:
                return
            for c in range(nch):
                if sample:
                    sample_attention()
                    break
                first = seq_first and c == 0
                blks = [1] if first else [0, 1]
                for kv in range(4):
                    for bl in blks:
                        kslot = c + bl
                        (bk,) = nb()
                        P.op("pe", lambda e, kv=kv, kslot=kslot, bk=bk, c=c: e.matmul(
                            ps[:, bk, :].rearrange("p (h t) -> p h t", h=4), lhsT=kTs[:, kv, kslot * 128:(kslot + 1) * 128],
                            rhs=qTs[:, 4 * kv:4 * kv + 4, c * 128:(c + 1) * 128], start=True, stop=False),
                             reads=[B("kTs", kslot)] + [B("QKO", 4 * kv + i, c) for i in range(4)], writes=[PB(bk)], signal=False)
                        P.op("pe", lambda e, bl=bl, bk=bk: e.matmul(ps[:, bk, :], lhsT=ident[:], rhs=amask_t[:, bl, :], start=False, stop=True),
                             reads=CB, writes=[PB(bk)])
                        u = 4 * bl + kv
                        P.op("act", lambda e, bk=bk, u=u: e.activation(out=RA[:, u * 512:(u + 1) * 512], in_=ps[:, bk, :], func=AF.Exp, scale=0.125),
                             reads=[PB(bk)], writes=[B("RA", u)])
                obk = nb(4)
                for h in range(16):
                    kv = h // 4
                    for bi_, bl in enumerate(blks):
                        kslot = c + bl
                        u = 4 * bl + kv
                        P.op("pe", lambda e, h=h, kv=kv, u=u, kslot=kslot, bi_=bi_, nbl=len(blks), ob4=obk[h // 4]: e.matmul(
                            ps[:, ob4, (h % 4) * 128:(h % 4) * 128 + 65],
                            lhsT=RA[:, u * 512 + (h % 4) * 128:u * 512 + (h % 4 + 1) * 128], rhs=vaug[:, kslot, kv, :],
                            start=(bi_ == 0), stop=(bi_ == nbl - 1)),
                             reads=[B("RA", u), B("vaug", kslot)], writes=[PB(obk[h // 4])], signal=(bi_ == len(blks) - 1 and h % 4 == 3))
                attn_finish(nt, c, obk)
            if SSTOP < 3:
                return
            if not sample and not seq_last:
                P.op("pool", lambda e: e.tensor_copy(kTs[:, :, 0:128], kTs[:, :, 512:640]), reads=[B("kTs", 4)], writes=[B("kTs", 0)])
                P.op("pool", lambda e: e.tensor_copy(vaug[:, 0, :, :], vaug[:, 4, :, :]), reads=[B("vaug", 4)], writes=[B("vaug", 0)])
            dense_out_phase_named(nt, nch, xTa, lambda fk, c: B("xTa", c), 1, 2)

        def attn_finish(nt, c, obk):
            den = stat[0:nt, 32:48]
            for q4 in range(4):
                P.op("dve", lambda e, q4=q4: e.tensor_tensor(
                    out=stat[0:nt, 32 + 4 * q4:36 + 4 * q4], in0=ps[0:nt, obk[q4], :].rearrange("p (h d) -> p h d", h=4)[:, :, 64],
                    in1=sexp_t[0:nt, 4 * q4:4 * q4 + 4], op=ALU.add),
                     reads=[PB(obk[q4])] + CB, writes=[B("den", q4)])
            P.op("dve", lambda e: e.reciprocal(den, den), reads=[B("den", i) for i in range(4)], writes=[B("rden")])
            for q4 in range(4):
                P.op("dve", lambda e, q4=q4: e.tensor_tensor(
                    out=ob[0:nt, q4 * 256:(q4 + 1) * 256].rearrange("p (h d) -> p h d", h=4),
                    in0=ps[0:nt, obk[q4], :].rearrange("p (h d) -> p h d", h=4)[:, :, 0:64],
                    in1=stat[0:nt, 32 + 4 * q4:36 + 4 * q4].unsqueeze(2).to_broadcast([nt, 4, 64]), op=ALU.mult),
                     reads=[PB(obk[q4]), B("rden")], writes=[B("ob")])
            (tbk,) = nb()
            psb = ps[:, tbk, :].bitcast(BF16)
            for k in range(8):
                P.op("pe", lambda e, k=k, psb=psb: e.transpose(psb[:, k * nt:(k + 1) * nt], ob[0:nt, k * 128:(k + 1) * 128], ident[0:nt, 0:nt]),
                     reads=[B("ob")] + CB, writes=[PB(tbk)], signal=(k == 7))
            P.op("act", lambda e, psb=psb, c=c: e.activation(out=xTa[:, :, c * nt:(c + 1) * nt],
                                                             in_=psb[:, 0:8 * nt].rearrange("p (k t) -> p k t", k=8), func=AF.Copy),
                 reads=[PB(tbk)], writes=[B("xTa", c)])

        def rope_tok(buf, nh, nt, rc, rsn, bkey):
            v = buf[0:nt, 0:nh * 64].rearrange("p (h d) -> p h d", h=nh)
            x1, x2 = v[:, :, 0:8], v[:, :, 8:16]
            cb = rc.unsqueeze(1).to_broadcast([nt, nh, 8])
            sbb = rsn.unsqueeze(1).to_broadcast([nt, nh, 8])
            t = [atmp[0:nt, i, 0:nh, :] for i in range(4)]
            bb_ = [B(*bkey), B("atmp")] + CB
            P.op("dve", lambda e: e.tensor_tensor(out=t[0], in0=x1, in1=cb, op=ALU.mult), reads=bb_, writes=[B("atmp")])
            P.op("dve", lambda e: e.tensor_tensor(out=t[1], in0=x2, in1=sbb, op=ALU.mult), reads=bb_, writes=[B("atmp")])
            P.op("dve", lambda e: e.tensor_tensor(out=t[2], in0=x2, in1=cb, op=ALU.mult), reads=bb_, writes=[B("atmp")])
            P.op("dve", lambda e: e.tensor_tensor(out=t[3], in0=x1, in1=sbb, op=ALU.mult), reads=bb_, writes=[B("atmp")])
            P.op("dve", lambda e: e.tensor_tensor(out=x1, in0=t[0], in1=t[1], op=ALU.subtract), reads=[B("atmp")], writes=[B(*bkey)])
            P.op("dve", lambda e: e.tensor_tensor(out=x2, in0=t[2], in1=t[3], op=ALU.add), reads=[B("atmp")], writes=[B(*bkey)])

        lane_ck = [P.lane(), P.lane()]
        lane_cv = [P.lane(), P.lane()]
        lane_am = [P.lane(), P.lane()]
        lane_kw = [P.lane(), P.lane()]
        lane_vw = [P.lane(), P.lane()]

        def sample_attention():
            nt = NSTOK
            qTs = QKO[0:64, :, :]
            vc = Sf[:].rearrange("p a b -> p (a b)").bitcast(BF16)[:, 0:NSB * 260].rearrange("p (b h d) -> p b h d", b=NSB, h=4)
            P.op("pool", lambda e: e.memset(Sf[:].rearrange("p a b -> p (a b)").bitcast(BF16)[:, 0:NSB * 260], 1.0),
                 writes=[B("Sf", i) for i in range(8)])
            for kvp in range(2):
                (bk,) = nb()
                for kvi in range(2):
                    kv = 2 * kvp + kvi
                    P.op("pe", lambda e, kv=kv, kvi=kvi, bk=bk: e.matmul(
                        ps[0:nt, bk, kvi * 256:(kvi + 1) * 256].rearrange("p (h t) -> p h t", h=4), lhsT=kTs[:, kv, 128:128 + nt],
                        rhs=qTs[:, 4 * kv:4 * kv + 4, 0:nt], start=True, stop=False),
                         reads=[B("kTs", 1)] + [B("QKO", 4 * kv + i, 0) for i in range(4)], writes=[PB(bk)], signal=False)
                    P.op("pe", lambda e, kvi=kvi, bk=bk: e.matmul(ps[0:nt, bk, kvi * 256:(kvi + 1) * 256], lhsT=ident[0:nt, 0:nt],
                                                                  rhs=amask_t[0:nt, 0, 256:512], start=False, stop=True),
                         reads=CB, writes=[PB(bk)])
                P.op("act", lambda e, bk=bk, kvp=kvp: e.activation(out=xn[0:nt, 0, kvp * 512:(kvp + 1) * 512], in_=ps[0:nt, bk, :],
                                                                   func=AF.Exp, scale=0.125),
                     reads=[PB(bk)], writes=[B("xn", 0)])
            for b in range(NSB):
                i2 = b % 2
                ckf = qf[:, i2 * 512:i2 * 512 + 256]
                cvf = qf[:, i2 * 512 + 256:i2 * 512 + 512]
                al = [B("qf", i2)] if b < 2 else []
                P.dma("sp", ckf, ck_in[b], lane_ck[i2], writes=[B("ckf", i2)] + al)
                P.dma("sp", cvf, cv_in[b], lane_cv[i2], writes=[B("cvf", i2)] + al)
                P.dma("sp", amask_t[:, i2, 0:256], T["amask_c"][b], lane_am[i2], writes=[B("amc", i2)])
                out_tokens.append(P.dma("sp", kws[b, 0:124, :], qf[4:128, i2 * 512:i2 * 512 + 256], lane_kw[i2], reads=[B("ckf", i2)]))
                out_tokens.append(P.dma("sp", vws[b, 0:124, :], qf[4:128, i2 * 512 + 256:i2 * 512 + 512], lane_vw[i2], reads=[B("cvf", i2)]))
                P.op("pool", lambda e, i2=i2, ckf=ckf: e.tensor_copy(qb[:, i2 * 256:(i2 + 1) * 256], ckf),
                     reads=[B("ckf", i2)], writes=[B("qbc", i2)] + ([B("qb", 0)] if b < 2 else []))
                (tbk,) = nb()
                psb = ps[:, tbk, :].bitcast(BF16)
                for kv in range(4):
                    P.op("pe", lambda e, kv=kv, psb=psb, i2=i2: e.transpose(psb[0:64, kv * 128:(kv + 1) * 128],
                                                                            qb[:, i2 * 256 + kv * 64:i2 * 256 + (kv + 1) * 64], ident[:]),
                         reads=[B("qbc", i2)] + CB, writes=[PB(tbk)], signal=(kv == 3))
                P.op("dve", lambda e, psb=psb, i2=i2: e.tensor_copy(kTs[:, :, 256 + i2 * 128:256 + (i2 + 1) * 128],
                                                                    psb[0:64, 0:512].rearrange("p (h t) -> p h t", h=4)),
                     reads=[PB(tbk)], writes=[B("kTc", i2)] + ([B("kTs", 2 + i2)] if b < 2 else []))
                P.op("pool", lambda e, b=b, cvf=cvf: e.tensor_copy(vc[:, b, :, 0:64], cvf.rearrange("p (h d) -> p h d", h=4)),
                     reads=[B("cvf", i2)] + [B("Sf", i) for i in range(8)], writes=[B("vc", b)])
                for kvp in range(2):
                    (bk,) = nb()
                    for kvi in range(2):
                        kv = 2 * kvp + kvi
                        P.op("pe", lambda e, kv=kv, kvi=kvi, bk=bk, i2=i2: e.matmul(
                            ps[:, bk, kvi * 256:(kvi + 1) * 256].rearrange("p (h t) -> p h t", h=4),
                            lhsT=kTs[:, kv, 256 + i2 * 128:256 + (i2 + 1) * 128], rhs=qTs[:, 4 * kv:4 * kv + 4, 0:nt], start=True, stop=False),
                             reads=[B("kTc", i2)] + [B("QKO", 4 * kv + i, 0) for i in range(4)], writes=[PB(bk)], signal=False)
                        P.op("pe", lambda e, kvi=kvi, bk=bk, i2=i2: e.matmul(ps[:, bk, kvi * 256:(kvi + 1) * 256], lhsT=ident[:],
                                                                             rhs=amask_t[:, i2, 0:256], start=False, stop=True),
                             reads=[B("amc", i2)] + CB, writes=[PB(bk)])
                    u = 2 * b + kvp
                    P.op("act", lambda e, bk=bk, u=u: e.activation(out=RA[:, u * 512:(u + 1) * 512], in_=ps[:, bk, :], func=AF.Exp, scale=0.125),
                         reads=[PB(bk)], writes=[B("RA", u)])
            obk = nb(4)
            for h in range(16):
                kv = h // 4
                bank = obk[h // 4]
                for b in range(NSB):
                    co = (2 * b + kv // 2) * 512 + (kv % 2) * 256 + (h % 4) * 64
                    P.op("pe", lambda e, h=h, b=b, kv=kv, co=co, bank=bank: e.matmul(
                        ps[0:nt, bank, (h % 4) * 128:(h % 4) * 128 + 65], lhsT=RA[:, co:co + nt], rhs=vc[:, b, kv, :],
                        start=(b == 0), stop=False),
                         reads=[B("RA", co // 512), B("vc", b)], writes=[PB(bank)], signal=False)
                P.op("pe", lambda e, h=h, kv=kv, bank=bank: e.matmul(
                    ps[0:nt, bank, (h % 4) * 128:(h % 4) * 128 + 65], lhsT=xn[0:nt, 0, h * 64:(h + 1) * 64], rhs=vaug[0:nt, 1, kv, :],
                    start=False, stop=True),
                     reads=[B("xn", 0), B("vaug", 1)], writes=[PB(bank)])
            attn_finish(nt, 0, obk)

        tile_no = 0
        for sq in range(NSEQ):
            for ti in range(NTPS):
                r0 = sq * SEQ + ti * 512
                ri = tile_no % 2
                P.dma("sp", ropeR[:, ri], T["rope_r"][ti], lane_rope[ri], writes=[B("rope")])
                for c in range(4):
                    P.dma("sp", hb[:, c, :], xp[r0 + c * 128:r0 + (c + 1) * 128, :], lane_h[c], writes=[B("h", c)])
                import os
                KSTOP = int(os.environ.get("KSTOP", "99"))
                if KSTOP >= 1:
                    ret_layer(128, 4, False, ti == 0, ti == NTPS - 1, sq, ropeR[:, ri], dq_t, maskr_t, kdec_t)
                if KSTOP >= 2:
                    ffn_phase(128, 4, 0, 1, 1)
                if KSTOP >= 3:
                    swa_layer(128, 4, False, ti == 0, ti == NTPS - 1, sq, ti * 4)

                def store(c, r0=r0):
                    out_tokens.append(P.dma("sp", yp[r0 + c * 128:r0 + (c + 1) * 128, :], hb[:, c, :], lane_y[c], reads=[B("h", c)]))

                if KSTOP >= 4:
                    ffn_phase(128, 4, 1, 4, 3, after_chunk=store)
                else:
                    for c in range(4):
                        store(c)
                tile_no += 1
                if tile_no >= int(os.environ.get("KTILES", "99")):
                    break
            if tile_no >= int(os.environ.get("KTILES", "99")):
                break

        if do_sample:
            nts = NSTOK
            P.dma("sp", dq_t[:, :, 0:nts], T["dqs"], lane_c, writes=[B("c")])
            P.dma("sp", maskr_t[0:nts, :, 0:nts], T["maskrs"], lane_c, writes=[B("c")])
            P.dma("sp", kdec_t[0:nts, :], T["kdecs"], lane_c, writes=[B("c")])
            P.dma("sp", amask_t[0:nts, 0, 256:512], T["amask_n"], lane_c, writes=[B("c")])
            P.dma("sp", bmq_t[:], T["bmq"], lane_c, writes=[B("c")])
            P.dma("sp", stat[0:nts, 48:48 + NSB], T["bmrow"], lane_c, writes=[B("c")])
            P.dma("sp", ropeAs[0:nts], T["rope_as"], lane_c, writes=[B("c")])
            ri = tile_no % 2
            P.dma("sp", ropeR[:, ri, :, 0:nts], T["rope_rs"], lane_rope[ri], writes=[B("rope")])
            P.dma("sp", hb[0:nts, 0, :], xs, lane_h[0], writes=[B("h", 0)])
            ret_layer(nts, 1, True, False, False, 0, ropeR[:, ri], dq_t[:, :, 0:nts], maskr_t, kdec_t)

            def store_s(c):
                out_tokens.append(P.dma("sp", ys[:, :], hb[0:nts, 0, :], lane_y[0], reads=[B("h", 0)]))

            DBGS = int(os.environ.get("DBGS", "0"))
            if DBGS == 1:
                store_s(0)
            else:
                ffn_phase(nts, 1, 0, 1, 1)
                if DBGS == 2:
                    store_s(0)
                else:
                    swa_layer(nts, 1, True, True, True, 0, 0)
                    ffn_phase(nts, 1, 1, 4, 3, after_chunk=store_s)

        P.wait_tokens("sp", out_tokens)
        P.wait_tokens("sp", [(l.key, l.val) for l in lanes_ring + lanes_scr + lane_h + lane_y + lane_rope + lane_sf + lane_so2 + lane_ck + lane_cv + lane_am + lane_kw + lane_vw + [lane_c, lane_g, lane_so, lane_kv, lane_vv] if l.val > 0])
        P.emit(block)
    return nc


def kernel(x_prompt, x_sample, state_ret, cache_k_win, cache_v_win,
           ret_norm_pre, ret_w_in, ret_w_out, ret_norm_post, kv_norm, w_kv,
           swa_norm_pre, swa_w_q, swa_sinks, swa_w_o, swa_norm_post,
           ffn_norm_pre, ffn_w1, ffn_w2, ffn_norm_post, _ncores=8, _do_sample=True):
    f = lambda a: np.ascontiguousarray(np.asarray(a, dtype=np.float32))
    x_prompt, x_sample = f(x_prompt), f(x_sample)
    BATCH, SEQ, D = x_prompt.shape
    DB = x_sample.shape[0]
    n = _ncores
    NSEQ = BATCH // n
    NSB = DB // n
    tabs = host_tables(SEQ, NSB)
    nc = build(NSEQ, SEQ, NSB, tabs, do_sample=_do_sample)
    gcol = np.stack([f(ret_norm_pre)[0], f(ffn_norm_pre)[0], f(kv_norm), f(swa_norm_pre)[0], f(ffn_norm_pre)[1],
                     f(ffn_norm_pre)[1]], 0)
    grow = np.stack([np.broadcast_to(v, (128, 1024)) for v in
                     (f(ret_norm_post)[0], f(ffn_norm_post)[0], f(swa_norm_post)[0], f(ffn_norm_post)[1])], 0)
    shared = {
        "w_in": f(ret_w_in)[0], "w_out": f(ret_w_out)[0], "w_kv": f(w_kv), "w_q": f(swa_w_q)[0], "w_o": f(swa_w_o)[0],
        "w1": f(ffn_w1), "w2": f(ffn_w2), "gcol": np.ascontiguousarray(gcol), "grow": np.ascontiguousarray(grow),
        "sinks": np.ascontiguousarray(np.broadcast_to(f(swa_sinks)[0], (128, 16))),
    }
    for k, v in tabs.items():
        shared["t_" + k] = np.ascontiguousarray(v)
    st = f(state_ret)[0]
    ck, cv = f(cache_k_win), f(cache_v_win)
    in_maps = []
    for i in range(n):
        m = dict(shared)
        m["xp"] = x_prompt[i * NSEQ:(i + 1) * NSEQ].reshape(NSEQ * SEQ, D)
        m["xs"] = x_sample[i * NSB:(i + 1) * NSB].reshape(NSB * 4, D)
        m["st_in"] = st[i * NSB:(i + 1) * NSB]
        m["ck_in"] = ck[i * NSB:(i + 1) * NSB].reshape(NSB, 128, 256)
        m["cv_in"] = cv[i * NSB:(i + 1) * NSB].reshape(NSB, 128, 256)
        in_maps.append(m)
    res = run_bass_kernel_spmd(nc, in_maps, core_ids=list(range(n)))
    R = res.results
    cat = lambda k: np.concatenate([np.asarray(r[k]) for r in R], 0)
    y_prompt = cat("yp").reshape(BATCH, SEQ, D)
    y_sample = cat("ys").reshape(DB, 4, D)
    sp_ = cat("sp_o")[None]
    ss_ = cat("ss_o")[None]
    kwp_ = cat("kwp").reshape(BATCH, 128, 4, 64)
    vwp_ = cat("vwp").reshape(BATCH, 128, 4, 64)
    kws_ = cat("kws").reshape(DB, 128, 4, 64)
    vws_ = cat("vws").reshape(DB, 128, 4, 64)
    return (y_prompt, y_sample, sp_, ss_, kwp_, vwp_, kws_, vws_)
```

```python
from contextlib import ExitStack
import numpy as np
import ml_dtypes
import concourse.bass as bass
import concourse.mybir as mybir
from concourse.bass_utils import run_bass_kernel_spmd

F32 = mybir.dt.float32
BF16 = mybir.dt.bfloat16
ALU = mybir.AluOpType
AF = mybir.ActivationFunctionType

ENGS = ("pe", "act", "dve", "pool", "sp")
EPS = 1e-6
PAST = 16384
NS = 4


class Buf:
    __slots__ = ("name", "w", "r", "x")

    def __init__(self, name, x=False):
        self.name = name
        self.w = None
        self.r = {}
        self.x = x


class Lane:
    __slots__ = ("key", "sem", "val")

    def __init__(self, key, sem):
        self.key = key
        self.sem = sem
        self.val = 0


class Prog:
    def __init__(self, nc, stack):
        self.nc = nc
        self.stack = stack
        self.ops = {e: [] for e in ENGS}
        self.cnt = {e: 0 for e in ENGS}
        self.sems = {}
        for e in ENGS:
            self.sems[e] = stack.enter_context(nc.semaphore("s_" + e))
        self.seen = {e: {} for e in ENGS}
        self.nlanes = 0

    def lane(self):
        key = "L%d" % self.nlanes
        self.nlanes += 1
        sem = self.stack.enter_context(self.nc.semaphore(key))
        self.sems[key] = sem
        return Lane(key, sem)

    def _need(self, eng, reads, writes):
        need = {}
        for b in reads:
            if b.w is not None and need.get(b.w[0], 0) < b.w[1]:
                need[b.w[0]] = b.w[1]
            if b.x:
                for k, v in b.r.items():
                    if k != eng and need.get(k, 0) < v:
                        need[k] = v
        for b in writes:
            if b.w is not None and need.get(b.w[0], 0) < b.w[1]:
                need[b.w[0]] = b.w[1]
            for k, v in b.r.items():
                if need.get(k, 0) < v:
                    need[k] = v
        seen = self.seen[eng]
        waits = []
        for k, v in need.items():
            if k == "pe" and eng == "pe":
                continue
            if seen.get(k, 0) < v:
                seen[k] = v
                waits.append((k, v))
        return waits

    def _mark(self, tok, reads, writes):
        for b in reads:
            if b.r.get(tok[0], 0) < tok[1]:
                b.r[tok[0]] = tok[1]
        for b in writes:
            b.w = tok
            b.r = {}

    def op(self, eng, fn, reads=(), writes=(), signal=True):
        waits = self._need(eng, reads, writes)
        if signal:
            self.cnt[eng] += 1
            tok = (eng, self.cnt[eng])
        else:
            tok = (eng, self.cnt[eng] + 1)
        self._mark(tok, reads, writes)
        self.ops[eng].append((waits, fn, 1 if signal else 0, None))
        return tok

    def dma(self, q, out, in_, lane, reads=(), writes=(), **kw):
        waits = self._need(q, reads, writes)
        lane.val += 16
        tok = (lane.key, lane.val)
        self._mark(tok, reads, writes)

        def fn(e, out=out, in_=in_, kw=kw):
            return e.dma_start(out=out, in_=in_, **kw)

        self.ops[q].append((waits, fn, 0, lane))
        return tok

    def wait_tokens(self, eng, toks):
        waits = []
        for k, v in toks:
            if self.seen[eng].get(k, 0) < v:
                self.seen[eng][k] = v
                waits.append((k, v))
        self.ops[eng].append((waits, None, 0, None))

    def emit(self, block):
        sems = self.sems

        def run(ename, e):
            own = sems[ename]
            for waits, fn, sig, lane in self.ops[ename]:
                for k, v in waits:
                    e.wait_ge(sems[k], v)
                if fn is None:
                    continue
                ins = fn(e)
                if lane is not None:
                    ins.then_inc(lane.sem, 16)
                elif sig:
                    ins.then_inc(own, 1)

        @block.tensor
        def _(e):
            run("pe", e)

        @block.scalar
        def _(e):
            run("act", e)

        @block.vector
        def _(e):
            run("dve", e)

        @block.gpsimd
        def _(e):
            run("pool", e)

        @block.sync
        def _(e):
            run("sp", e)


def _gdec():
    return [1.0 - 2.0 ** (-5.0 - h) for h in range(4)]


def host_tables(SEQ, NSB):
    t = {}
    NTPS = SEQ // 512
    inv = (1.0 / (np.float32(10000.0) ** np.linspace(0.0, 1.0, 128, dtype=np.float32))).astype(np.float32)
    pos = np.arange(SEQ, dtype=np.float32)
    ang = (inv[:, None] * pos[None, :]).astype(np.float32)
    cs = np.stack([np.cos(ang), np.sin(ang)], axis=1).astype(np.float32)
    t["rope_r"] = np.ascontiguousarray(cs.reshape(128, 2, NTPS, 512).transpose(2, 0, 1, 3))
    pos_s = (np.float32(PAST) + np.arange(4, dtype=np.float32)).astype(np.float32)
    ang_s = (inv[:, None] * pos_s[None, :]).astype(np.float32)
    cs_s = np.stack([np.cos(ang_s), np.sin(ang_s)], axis=1).astype(np.float32)
    t["rope_rs"] = np.ascontiguousarray(np.tile(cs_s, (1, 1, NSB)))
    g = _gdec()
    i128 = np.arange(128, dtype=np.float64)
    dq = np.stack([np.power(g[h], i128 + 1) for h in range(4)], 0)
    t["dq"] = np.ascontiguousarray(np.broadcast_to(dq[None], (128, 4, 128))).astype(np.float32)
    mk = np.zeros((128, 4, 128), np.float64)
    for h in range(4):
        mk[:, h, :] = (i128[:, None] <= i128[None, :]) * np.power(g[h], -(i128[:, None] + 1)) / 16.0
    t["maskr"] = mk.astype(np.float32)
    t["kdec"] = np.stack([np.power(g[h], 127 - i128) / 16.0 for h in range(4)], 1).astype(np.float32)
    n = 4 * NSB
    tt = np.arange(n) % 4
    bb = np.arange(n) // 4
    dqs = np.stack([np.power(g[h], tt + 1.0) for h in range(4)], 0)
    t["dqs"] = np.ascontiguousarray(np.broadcast_to(dqs[None], (128, 4, n))).astype(np.float32)
    mks = np.zeros((n, 4, n), np.float64)
    for h in range(4):
        mks[:, h, :] = ((bb[:, None] == bb[None, :]) & (tt[:, None] <= tt[None, :])) * \
            np.power(g[h], -(tt[:, None] + 1.0)) / 16.0
    t["maskrs"] = mks.astype(np.float32)
    t["kdecs"] = np.stack([np.power(g[h], 3.0 - tt) / 16.0 for h in range(4)], 1).astype(np.float32)
    bm = (bb[:, None] == np.arange(NSB)[None, :]).astype(np.float32)
    t["bmrow"] = bm
    bmq = np.zeros((128, NSB, n), np.float32)
    bmq[:, bb, np.arange(n)] = 1.0
    t["bmq"] = bmq.astype(ml_dtypes.bfloat16)
    inv8 = (np.float32(500000.0) ** (-np.arange(8, dtype=np.float32) / np.float32(8.0))).astype(np.float32)
    anga = (pos[:, None] * inv8[None, :]).astype(np.float32)
    ca = np.stack([np.cos(anga), np.sin(anga)], 1).astype(np.float32)
    t["rope_a"] = np.ascontiguousarray(ca.reshape(SEQ // 128, 128, 2, 8).transpose(1, 0, 2, 3))
    angas = (pos_s[:, None] * inv8[None, :]).astype(np.float32)
    cas = np.stack([np.cos(angas), np.sin(angas)], 1).astype(np.float32)
    t["rope_as"] = np.ascontiguousarray(np.tile(cas, (NSB, 1, 1)))
    NEG = -30000.0
    kk = np.arange(128)
    prev = np.where(kk[:, None] > kk[None, :], 0.0, NEG)
    cur = np.where(kk[:, None] <= kk[None, :], 0.0, NEG)
    am = np.stack([np.tile(prev, (1, 4)), np.tile(cur, (1, 4))], 0)
    t["amask"] = am.astype(ml_dtypes.bfloat16)
    amc = np.full((NSB, 128, n), NEG)
    for b in range(NSB):
        for tq in range(4):
            amc[b, :, 4 * b + tq] = np.where(kk > tq, 0.0, NEG)
    t["amask_c"] = np.ascontiguousarray(np.tile(amc, (1, 1, 4))).astype(ml_dtypes.bfloat16)
    amn = np.where((bb[:, None] == bb[None, :]) & (tt[:, None] <= tt[None, :]), 0.0, NEG)
    t["amask_n"] = np.ascontiguousarray(np.tile(amn, (1, 4))).astype(ml_dtypes.bfloat16)
    return t


TABLE_DT = {"bmq": BF16, "amask": BF16, "amask_c": BF16, "amask_n": BF16}


def build(NSEQ, SEQ, NSB, tabs, do_sample=True):
    NTPS = SEQ // 512
    NSTOK = 4 * NSB
    nc = bass.Bass("TRN2", target_bir_lowering=False)

    def din(name, shape, dt=F32):
        return nc.dram_tensor(name, list(shape), dt, kind="ExternalInput").ap()

    def dout(name, shape):
        return nc.dram_tensor(name, list(shape), F32, kind="ExternalOutput").ap()

    xp = din("xp", [NSEQ * SEQ, 1024])
    xs = din("xs", [NSTOK, 1024])
    st_in = din("st_in", [NSB, 4, 256, 512])
    ck_in = din("ck_in", [NSB, 128, 256])
    cv_in = din("cv_in", [NSB, 128, 256])
    w_in = din("w_in", [1024, 6144])
    w_out = din("w_out", [2048, 1024])
    w_kv = din("w_kv", [1024, 512])
    w_q = din("w_q", [1024, 1024])
    w_o = din("w_o", [1024, 1024])
    w1 = din("w1", [2, 1024, 4096])
    w2 = din("w2", [2, 4096, 1024])
    gcol = din("gcol", [6, 1024])
    grow = din("grow", [4, 128, 1024])
    sinks = din("sinks", [128, 16])
    T = {k: din("t_" + k, v.shape, TABLE_DT.get(k, F32)) for k, v in tabs.items()}

    yp = dout("yp", [NSEQ * SEQ, 1024])
    ys = dout("ys", [NSTOK, 1024])
    sp_o = dout("sp_o", [NSEQ, 4, 256, 512])
    ss_o = dout("ss_o", [NSB, 4, 256, 512])
    kwp = dout("kwp", [NSEQ, 128, 256])
    vwp = dout("vwp", [NSEQ, 128, 256])
    kws = dout("kws", [NSB, 128, 256])
    vws = dout("vws", [NSB, 128, 256])

    slabs = []

    def add(W, r0, c0):
        slabs.append(W[r0:r0 + 1024, c0:c0 + 512].rearrange("(k p) n -> p k n", p=128))

    for s in range(12):
        add(w_in, 0, 512 * s)
    for ch in range(2):
        for kh in range(2):
            add(w_out, 1024 * kh, 512 * ch)

    def add_ffn(l):
        for s in range(8):
            add(w1[l], 0, 512 * s)
        for ch in range(2):
            for kq in range(4):
                add(w2[l], 1024 * kq, 512 * ch)

    add_ffn(0)
    add(w_kv, 0, 0)
    for ch in range(2):
        add(w_q, 0, 512 * ch)
    for ch in range(2):
        add(w_o, 0, 512 * ch)
    add_ffn(1)
    NSL = len(slabs)
    scr = nc.dram_tensor("wscr", [NSL, 128, 4096], BF16, kind="ExternalOutput").ap()


    n_tiles = NSEQ * NTPS + (1 if do_sample else 0)
    total_slabs = n_tiles * NSL

    with ExitStack() as st:
        P = Prog(nc, st)

        def sb(name, shape, dt):
            return st.enter_context(nc.sbuf_tensor(name, list(shape), dt))

        bufs = {}

        def B(*key):
            b = bufs.get(key)
            if b is None:
                b = bufs[key] = Buf(str(key), x=(key[0] == "ps"))
            return b

        hb = sb("h", [128, 4, 1024], F32)
        xTa = sb("xTa", [128, 8, 512], BF16)
        xTb = sb("xTb", [128, 8, 512], BF16)
        QKO = sb("QKO", [128, 16, 512], BF16)
        RA = sb("RA", [128, 16384], BF16)
        khat = sb("khat", [128, 2, 1024], BF16)
        Sf = sb("Sf", [128, 8, 512], F32)
        Sb = sb("Sb", [128, 8, 512], BF16)
        og = sb("og", [128, 2048], BF16)
        ring = sb("ring", [128, NS, 4096], BF16)
        ropeR = sb("ropeR", [128, 2, 2, 512], F32)
        gbuf = sb("gbuf", [128, 1024], F32)
        xn = sb("xn", [128, 2, 1024], BF16)
        junk = sb("junk", [128, 1024], BF16)
        rtmp = sb("rtmp", [128, 4, 512], F32)
        scb = sb("scb", [128, 2, 128], BF16)
        ident = sb("ident", [128, 128], BF16)
        identf = sb("identf", [128, 128], F32)
        dq_t = sb("dq_t", [128, 4, 128], F32)
        maskr_t = sb("maskr_t", [128, 4, 128], F32)
        kdec_t = sb("kdec_t", [128, 4], F32)
        amask_t = sb("amask_t", [128, 2, 512], BF16)
        gcol_t = sb("gcol_t", [128, 6, 8], F32)
        sexp_t = sb("sexp_t", [128, 16], F32)
        eps_t = sb("eps_t", [128, 1], F32)
        stat = sb("stat", [128, 64], F32)
        ropeA = sb("ropeA", [128, SEQ // 128, 2, 8], F32)
        ropeAs = sb("ropeAs", [128, 2, 8], F32)
        kf2 = sb("kf2", [128, 2, 256], F32)
        vf2 = sb("vf2", [128, 2, 256], F32)
        kb2 = sb("kb2", [128, 2, 256], BF16)
        kTs = sb("kTs", [64, 4, 5 * 128], BF16)
        vaug = sb("vaug", [128, 5, 4, 65], BF16)
        qf = sb("qf", [128, 1024], F32)
        qb = sb("qb", [128, 1024], BF16)
        atmp = sb("atmp", [128, 4, 16, 8], F32)
        ob = sb("ob", [128, 1024], BF16)
        rl = sb("rl", [128, 2, 512], F32)
        bmq_t = sb("bmq_t", [128, NSB, NSTOK], BF16)
        ps = st.enter_context(nc.psum_tensor("ps", [128, 8, 512], F32))
        block = st.enter_context(nc.Block())

        lanes_ring = [P.lane() for _ in range(NS)]
        lanes_ring_sw = [P.lane() for _ in range(NS)]
        lanes_scr = [P.lane() for _ in range(NS)]
        lane_h = [P.lane() for _ in range(4)]
        lane_y = [P.lane() for _ in range(4)]
        lane_c = P.lane()
        lane_rope = [P.lane() for _ in range(2)]
        lane_g = P.lane()
        lane_so = P.lane()
        lane_kv = P.lane()
        lane_vv = P.lane()
        out_tokens = []

        pstate = {}

        def nb(n=1, lo=0, hi=8):
            bi = pstate.get((lo, hi), 0)
            if n > 1 and bi % n:
                bi += n - bi % n
            w = hi - lo
            r = [lo + (bi + i) % w for i in range(n)]
            pstate[(lo, hi)] = (bi + n) % w
            return r

        def PB(i):
            return B("ps", i)

        import os
        KSKIP = os.environ.get("KSKIP", "")
        wst = {"next": 0, "issued": 0}

        def w_issue(g):
            if g >= total_slabs:
                return
            t_, i = divmod(g, NSL)
            slot = g % NS
            dst = ring[:, slot, :]
            if t_ == 0:
                P.dma("pool", dst.rearrange("p (k n) -> p k n", k=8), slabs[i], lanes_ring_sw[slot], writes=[B("ring", slot)])
                P.dma("sp", scr[i], dst, lanes_scr[slot], reads=[B("ring", slot)], writes=[B("scr", i)])
            else:
                P.dma("sp", dst, scr[i], lanes_ring[slot], reads=[B("scr", i)], writes=[B("ring", slot)])

        def w_get():
            g = wst["next"]
            wst["next"] += 1
            slot = g % NS
            return ring[:, slot, :].rearrange("p (k n) -> p k n", k=8), B("ring", slot), g

        def w_done(g):
            w_issue(g + NS)

        import os
        KSKIP = os.environ.get("KSKIP", "")
        if "w" not in KSKIP:
            for g in range(NS):
                w_issue(g)

        if "c" not in KSKIP:
          P.dma("sp", dq_t[:], T["dq"], lane_c, writes=[B("c")])
        if "c" not in KSKIP:
          P.dma("sp", maskr_t[:], T["maskr"], lane_c, writes=[B("c")])
        if "c" not in KSKIP:
          P.dma("sp", kdec_t[:], T["kdec"], lane_c, writes=[B("c")])
        if "c" not in KSKIP:
          P.dma("sp", amask_t[:], T["amask"].rearrange("b k q -> k b q"), lane_c, writes=[B("c")])
        if "g" not in KSKIP:
          P.dma("sp", gcol_t[:], gcol.rearrange("g (k p) -> p g k", p=128), lane_c, writes=[B("c")],
              allow_slow_non_contiguous=True)
        if "c" not in KSKIP:
          P.dma("sp", sexp_t[:], sinks, lane_c, writes=[B("c")])
        if "c" not in KSKIP:
          P.dma("sp", ropeA[:], T["rope_a"], lane_c, writes=[B("c")])
        P.op("pool", lambda e: e.memset(identf[:], 0.0), writes=[B("identf")])
        P.op("pool", lambda e: e.affine_select(out=identf[:], in_=identf[:], compare_op=ALU.not_equal, fill=1.0,
                                               base=0, pattern=[[-1, 128]], channel_multiplier=1),
             reads=[B("identf")], writes=[B("identf")])
        P.op("pool", lambda e: e.tensor_copy(ident[:], identf[:]), reads=[B("identf")], writes=[B("c2")])
        P.op("pool", lambda e: e.memset(eps_t[:], EPS), writes=[B("c2")])
        if "v" not in KSKIP:
          P.op("pool", lambda e: e.memset(vaug[:].rearrange("p a b c -> p (a b c)"), 1.0), writes=[B("vaug", i) for i in range(5)])
        P.op("act", lambda e: e.activation(out=sexp_t[:], in_=sexp_t[:], func=AF.Exp), reads=[B("c")], writes=[B("c")])
        CB = [B("c"), B("c2")]
        gdec = _gdec()

        def rstd_from_ms(ms_ap, out_ap, n, rds, wr):
            P.op("act", lambda e: e.activation(out=out_ap, in_=ms_ap, func=AF.Sqrt, bias=eps_t[0:n, 0:1], scale=1.0),
                 reads=rds + CB, writes=[wr])
            P.op("dve", lambda e: e.reciprocal(out_ap, out_ap), reads=[wr], writes=[wr])

        def norm_phase(nt, nch, dests):
            for c in range(nch):
                ms = stat[0:nt, c:c + 1]
                P.op("act", lambda e, c=c, ms=ms: e.activation(out=junk[0:nt, :], in_=hb[0:nt, c, :], func=AF.Square,
                                                               scale=1.0 / 32, accum_out=ms),
                     reads=[B("h", c)], writes=[B("junk"), B("ms", c)])
            rstd_from_ms(stat[0:nt, 0:nch], stat[0:nt, 4:4 + nch], nt, [B("ms", c) for c in range(nch)], B("rsall"))
            for c in range(nch):
                rs = stat[0:nt, 4 + c:5 + c]
                xi = c % 2
                P.op("act", lambda e, c=c, rs=rs, xi=xi: e.activation(out=xn[0:nt, xi, :], in_=hb[0:nt, c, :], func=AF.Copy, scale=rs),
                     reads=[B("h", c), B("rsall")], writes=[B("xn", xi)])
                (bk,) = nb()
                psb = ps[:, bk, :].bitcast(BF16)
                for k in range(8):
                    P.op("pe", lambda e, k=k, xi=xi, psb=psb: e.transpose(psb[:, k * nt:(k + 1) * nt], xn[0:nt, xi, k * 128:(k + 1) * 128], ident[0:nt, 0:nt]),
                         reads=[B("xn", xi)] + CB, writes=[PB(bk)], signal=(k == 7))
                for gi, dst, nm in dests:
                    P.op("dve", lambda e, gi=gi, dst=dst, c=c, psb=psb: e.tensor_tensor(
                        out=dst[:, :, c * nt:(c + 1) * nt], in0=psb[:, 0:8 * nt].rearrange("p (k t) -> p k t", k=8),
                        in1=gcol_t[:, gi, :].unsqueeze(2).to_broadcast([128, 8, nt]), op=ALU.mult),
                         reads=[PB(bk)] + CB, writes=[B(nm, c)])

        def load_g(gi):
            P.dma("sp", gbuf[:], grow[gi], lane_g, writes=[B("gbuf")])

        def ffn_phase(nt, nch, l, gi_pre, gi_post, after_chunk=None):
            TT = nt * nch
            norm_phase(nt, nch, [(gi_pre, xTa, "xTa")])
            uT = RA[:].rearrange("p (j t) -> p j t", j=32)
            ri = 0
            for s in range(8):
                wt, wb, g = w_get()
                for j in range(4):
                    (bk,) = nb()
                    for k in range(8):
                        P.op("pe", lambda e, k=k, j=j, wt=wt, bk=bk: e.matmul(
                            ps[:, bk, 0:TT], lhsT=wt[:, k, j * 128:(j + 1) * 128], rhs=xTa[:, k, 0:TT],
                            start=(k == 0), stop=(k == 7)),
                             reads=[B("xTa", c) for c in range(nch)] + [wb], writes=[PB(bk)], signal=(k == 7))
                    fj = 4 * s + j
                    r = ri % 2
                    ri += 1
                    P.op("act", lambda e, bk=bk, r=r: e.activation(out=rl[:, r, 0:TT], in_=ps[:, bk, 0:TT], func=AF.Relu),
                         reads=[PB(bk)], writes=[B("rl", r)])
                    P.op("pool", lambda e, fj=fj, r=r: e.tensor_tensor(out=uT[:, fj, 0:TT], in0=rl[:, r, 0:TT], in1=rl[:, r, 0:TT], op=ALU.mult),
                         reads=[B("rl", r)], writes=[B("RA", fj)])
                w_done(g)
            dense_out_phase_named(nt, nch, uT, lambda fk, c: B("RA", fk), 4, gi_post, after_chunk)

        def dense_out_phase_named(nt, nch, srcT, bufof, nkg, gi_post, after_chunk=None):
            load_g(gi_post)
            bank_of = {}
            for ch in range(2):
                bks = nb(4) if nch > 1 else nb(1)
                for kg in range(nkg):
                    wt, wb, g = w_get()
                    for c in range(nch):
                        for k in range(8):
                            fk = kg * 8 + k
                            P.op("pe", lambda e, c=c, k=k, fk=fk, wt=wt, bk=bks[c], kg=kg: e.matmul(
                                ps[0:nt, bk, :], lhsT=srcT[:, fk, c * nt:(c + 1) * nt], rhs=wt[:, k, :],
                                start=(kg == 0 and k == 0), stop=(kg == nkg - 1 and k == 7)),
                                 reads=[bufof(fk, c), wb], writes=[PB(bks[c])],
                                 signal=(k == 7))
                    w_done(g)
                for c in range(nch):
                    bank_of[(c, ch)] = bks[c]
                    ms = stat[0:nt, 8 + 2 * c + ch:9 + 2 * c + ch]
                    P.op("act", lambda e, c=c, ms=ms, bk=bks[c]: e.activation(out=junk[0:nt, 0:512], in_=ps[0:nt, bk, :], func=AF.Square,
                                                                             scale=1.0 / 32, accum_out=ms),
                         reads=[PB(bks[c])], writes=[B("junk"), B("ms2", c, ch)])
                    if ch == 0:
                        P.op("dve", lambda e, c=c, bk=bks[c]: e.tensor_tensor(out=rtmp[0:nt, c, :], in0=ps[0:nt, bk, :],
                                                                            in1=gbuf[0:nt, 0:512], op=ALU.mult),
                             reads=[PB(bks[c]), B("gbuf")], writes=[B("rt", c)])
            for c in range(nch):
                P.op("dve", lambda e, c=c: e.tensor_tensor(out=stat[0:nt, 16 + c:17 + c], in0=stat[0:nt, 8 + 2 * c:9 + 2 * c],
                                                           in1=stat[0:nt, 9 + 2 * c:10 + 2 * c], op=ALU.add),
                     reads=[B("ms2", c, 0), B("ms2", c, 1)], writes=[B("ms3", c)])
            rstd_from_ms(stat[0:nt, 16:16 + nch], stat[0:nt, 20:20 + nch], nt, [B("ms3", c) for c in range(nch)], B("rs3"))
            for c in range(nch):
                rs = stat[0:nt, 20 + c:21 + c]
                P.op("dve", lambda e, c=c, rs=rs: e.scalar_tensor_tensor(
                    out=hb[0:nt, c, 0:512], in0=rtmp[0:nt, c, :], scalar=rs, in1=hb[0:nt, c, 0:512],
                    op0=ALU.mult, op1=ALU.add),
                     reads=[B("rt", c), B("rs3"), B("h", c)], writes=[B("h", c)])
                bk = bank_of[(c, 1)]
                r2 = c % 2
                tmp = rl[0:nt, r2, :]
                P.op("dve", lambda e, bk=bk, tmp=tmp: e.tensor_tensor(out=tmp, in0=ps[0:nt, bk, :],
                                                                    in1=gbuf[0:nt, 512:1024], op=ALU.mult),
                     reads=[PB(bk), B("gbuf")], writes=[B("rl", r2)])
                P.op("dve", lambda e, c=c, tmp=tmp, rs=rs: e.scalar_tensor_tensor(
                    out=hb[0:nt, c, 512:1024], in0=tmp, scalar=rs, in1=hb[0:nt, c, 512:1024],
                    op0=ALU.mult, op1=ALU.add),
                     reads=[B("rl", r2), B("rs3"), B("h", c)], writes=[B("h", c)])
                if after_chunk is not None:
                    after_chunk(c)

        def ret_layer(nt, nch, sample, seq_first, seq_last, seq_idx, ropeT, dqT, maskT, kdecT):
            TT = nt * nch
            norm_phase(nt, nch, [(0, xTa, "xTa")])
            xa_r = [B("xTa", c) for c in range(nch)]
            import os
            RSTOP = int(os.environ.get("RSTOP", "99"))
            if RSTOP < 1:
                return
            for s in range(4):
                wt, wb, g = w_get()
                bks = nb(4)
                for j in range(4):
                    for k in range(8):
                        P.op("pe", lambda e, k=k, j=j, wt=wt, bk=bks[j]: e.matmul(
                            ps[:, bk, 0:TT], lhsT=wt[:, k, j * 128:(j + 1) * 128], rhs=xTa[:, k, 0:TT],
                            start=(k == 0), stop=(k == 7)),
                             reads=xa_r + [wb], writes=[PB(bks[j])], signal=(k == 7))
                w_done(g)
                for hh in range(2):
                    b1, b2 = bks[2 * hh], bks[2 * hh + 1]
                    f1 = 4 * s + 2 * hh
                    head = (f1 % 8) // 2
                    isq = s < 2
                    X1, X2 = ps[:, b1, 0:TT], ps[:, b2, 0:TT]
                    C_, S_ = ropeT[:, 0, 0:TT], ropeT[:, 1, 0:TT]
                    t = [rtmp[:, i, 0:TT] for i in range(4)]
                    P.op("dve", lambda e, X1=X1, C_=C_, t=t: e.tensor_tensor(out=t[0], in0=X1, in1=C_, op=ALU.mult),
                         reads=[PB(b1), B("rope")], writes=[B("rt", 0)])
                    P.op("dve", lambda e, X2=X2, S_=S_, t=t: e.tensor_tensor(out=t[1], in0=X2, in1=S_, op=ALU.mult),
                         reads=[PB(b2), B("rope")], writes=[B("rt", 1)])
                    P.op("dve", lambda e, X2=X2, C_=C_, t=t: e.tensor_tensor(out=t[2], in0=X2, in1=C_, op=ALU.mult),
                         reads=[PB(b2), B("rope")], writes=[B("rt", 2)])
                    P.op("dve", lambda e, X1=X1, S_=S_, t=t: e.tensor_tensor(out=t[3], in0=X1, in1=S_, op=ALU.mult),
                         reads=[PB(b1), B("rope")], writes=[B("rt", 3)])
                    w1b = [B("QKO", f1, c) for c in range(nch)]
                    w2b = [B("QKO", f1 + 1, c) for c in range(nch)]
                    if isq:
                        dqv = dqT[:, head, :].unsqueeze(1).to_broadcast([128, nch, nt])
                        P.op("pool", lambda e, t=t: e.tensor_tensor(out=t[0], in0=t[0], in1=t[1], op=ALU.subtract),
                             reads=[B("rt", 0), B("rt", 1)], writes=[B("rt", 0)])
                        P.op("pool", lambda e, t=t, f1=f1, dqv=dqv: e.tensor_tensor(
                            out=QKO[:, f1, 0:TT].rearrange("p (c t) -> p c t", c=nch), in0=t[0].rearrange("p (c t) -> p c t", c=nch),
                            in1=dqv, op=ALU.mult), reads=[B("rt", 0)] + CB, writes=w1b)
                        P.op("pool", lambda e, t=t: e.tensor_tensor(out=t[2], in0=t[2], in1=t[3], op=ALU.add),
                             reads=[B("rt", 2), B("rt", 3)], writes=[B("rt", 2)])
                        P.op("pool", lambda e, t=t, f1=f1, dqv=dqv: e.tensor_tensor(
                            out=QKO[:, f1 + 1, 0:TT].rearrange("p (c t) -> p c t", c=nch), in0=t[2].rearrange("p (c t) -> p c t", c=nch),
                            in1=dqv, op=ALU.mult), reads=[B("rt", 2)] + CB, writes=w2b)
                    else:
                        P.op("pool", lambda e, t=t, f1=f1: e.tensor_tensor(out=QKO[:, f1, 0:TT], in0=t[0], in1=t[1], op=ALU.subtract),
                             reads=[B("rt", 0), B("rt", 1)], writes=w1b)
                        P.op("pool", lambda e, t=t, f1=f1: e.tensor_tensor(out=QKO[:, f1 + 1, 0:TT], in0=t[2], in1=t[3], op=ALU.add),
                             reads=[B("rt", 2), B("rt", 3)], writes=w2b)
            if RSTOP < 2:
                return
            for s in range(8):
                wt, wb, g = w_get()
                hv = s % 4
                for c in range(nch):
                    (bk,) = nb()
                    for k in range(8):
                        P.op("pe", lambda e, k=k, c=c, wt=wt, bk=bk: e.matmul(
                            ps[0:nt, bk, :], lhsT=xTa[:, k, c * nt:(c + 1) * nt], rhs=wt[:, k, :],
                            start=(k == 0), stop=(k == 7)),
                             reads=[B("xTa", c), wb], writes=[PB(bk)], signal=(k == 7))
                    if s < 4:
                        u = 4 * c + hv
                        P.op("act", lambda e, bk=bk, u=u: e.activation(out=RA[0:nt, u * 512:(u + 1) * 512], in_=ps[0:nt, bk, :], func=AF.Copy),
                             reads=[PB(bk)], writes=[B("RA", u)])
                    else:
                        u = 16 + 4 * c + hv
                        P.op("act", lambda e, bk=bk, u=u: e.activation(out=RA[0:nt, u * 512:(u + 1) * 512], in_=ps[0:nt, bk, :], func=AF.Silu),
                             reads=[PB(bk)], writes=[B("RA", u)])
                w_done(g)
            if RSTOP < 3:
                return
            for c in range(nch):
                first = seq_first and c == 0
                last = seq_last and c == nch - 1
                cs = slice(c * nt, (c + 1) * nt)
                ki = c % 2
                if True:
                    (bk,) = nb(1, 4, 8)
                    psb = ps[:, bk, :].bitcast(BF16)
                    for kc in range(8):
                        P.op("pe", lambda e, kc=kc, psb=psb, cs=cs: e.transpose(psb[0:nt, kc * 128:(kc + 1) * 128], QKO[:, 8 + kc, cs], ident[:]),
                             reads=[B("QKO", 8 + kc, c)] + CB, writes=[PB(bk)], signal=(kc == 7))
                    for h in range(4):
                        P.op("dve", lambda e, h=h, psb=psb, ki=ki: e.tensor_scalar(
                            khat[0:nt, ki, h * 256:(h + 1) * 256], psb[0:nt, h * 256:(h + 1) * 256], kdecT[0:nt, h:h + 1], None, op0=ALU.mult),
                             reads=[PB(bk)] + CB, writes=[B("khat", ki, h)])
                obk = [0, 1, 2, 3]
                for h in range(4):
                    (bk,) = nb(1, 4, 8)
                    for kk in range(2):
                        P.op("pe", lambda e, h=h, kk=kk, bk=bk, cs=cs: e.matmul(
                            ps[0:nt, bk, 0:nt], lhsT=QKO[:, 8 + 2 * h + kk, cs], rhs=QKO[:, 2 * h + kk, cs],
                            start=(kk == 0), stop=(kk == 1)),
                             reads=[B("QKO", 8 + 2 * h + kk, c), B("QKO", 2 * h + kk, c)], writes=[PB(bk)], signal=(kk == 1))
                    si = h % 2
                    P.op("dve", lambda e, h=h, bk=bk, si=si: e.tensor_tensor(out=scb[0:nt, si, 0:nt], in0=ps[0:nt, bk, 0:nt],
                                                                           in1=maskT[0:nt, h, 0:nt], op=ALU.mult),
                         reads=[PB(bk)] + CB, writes=[B("scb", si)])
                    vu = 4 * c + h
                    ob_ = obk[h]
                    cross = (not first) and (not sample)
                    P.op("pe", lambda e, si=si, vu=vu, ob_=ob_, cross=cross: e.matmul(
                        ps[0:nt, ob_, :], lhsT=scb[0:nt, si, 0:nt], rhs=RA[0:nt, vu * 512:(vu + 1) * 512], start=True,
                        stop=(not cross and not sample)),
                         reads=[B("scb", si), B("RA", vu)], writes=[PB(ob_)], signal=(not cross))
                    if cross:
                        for kk in range(2):
                            P.op("pe", lambda e, h=h, kk=kk, ob_=ob_, cs=cs: e.matmul(
                                ps[0:nt, ob_, :], lhsT=QKO[:, 2 * h + kk, cs], rhs=Sb[:, 2 * h + kk, :], start=False, stop=(kk == 1)),
                                 reads=[B("QKO", 2 * h + kk, c), B("Sb", 2 * h + kk)], writes=[PB(ob_)], signal=(kk == 1))
                    ms = stat[0:nt, 24 + h:25 + h]
                    if not sample:
                        P.op("act", lambda e, ob_=ob_, ms=ms: e.activation(out=junk[0:nt, 0:512], in_=ps[0:nt, ob_, :], func=AF.Square,
                                                                            scale=float(512 ** -0.5), accum_out=ms),
                             reads=[PB(ob_)], writes=[B("junk"), B("mso", h)])
                    if not sample:
                        for kk in range(2):
                            (sbk,) = nb(1, 4, 8)
                            P.op("pe", lambda e, h=h, kk=kk, sbk=sbk, ki=ki, vu=vu: e.matmul(
                                ps[:, sbk, :], lhsT=khat[0:nt, ki, h * 256 + kk * 128:h * 256 + (kk + 1) * 128],
                                rhs=RA[0:nt, vu * 512:(vu + 1) * 512], start=True, stop=True),
                                 reads=[B("khat", ki, h), B("RA", vu)], writes=[PB(sbk)])
                            si_ = 2 * h + kk
                            if first:
                                P.op("dve", lambda e, sbk=sbk, si_=si_: e.tensor_copy(Sf[:, si_, :], ps[:, sbk, :]),
                                     reads=[PB(sbk)], writes=[B("Sf", si_)])
                            else:
                                P.op("dve", lambda e, sbk=sbk, si_=si_, h=h: e.scalar_tensor_tensor(
                                    out=Sf[:, si_, :], in0=Sf[:, si_, :], scalar=float(gdec[h] ** 128), in1=ps[:, sbk, :],
                                    op0=ALU.mult, op1=ALU.add),
                                     reads=[PB(sbk), B("Sf", si_)], writes=[B("Sf", si_)])
                            if not last:
                                P.op("act", lambda e, si_=si_: e.activation(out=Sb[:, si_, :], in_=Sf[:, si_, :], func=AF.Copy),
                                     reads=[B("Sf", si_)], writes=[B("Sb", si_)])
                if sample:
                    sample_cross(obk)
                    for h in range(4):
                        P.op("act", lambda e, h=h: e.activation(out=junk[0:nt, 0:512], in_=ps[0:nt, obk[h], :], func=AF.Square,
                                                                scale=float(512 ** -0.5), accum_out=stat[0:nt, 24 + h:25 + h]),
                             reads=[PB(obk[h])], writes=[B("junk"), B("mso", h)])
                rs4 = stat[0:nt, 28:32]
                rstd_from_ms(stat[0:nt, 24:28], rs4, nt, [B("mso", h) for h in range(4)], B("rso"))
                for h in range(4):
                    P.op("dve", lambda e, h=h, c=c: e.scalar_tensor_tensor(
                        out=og[0:nt, h * 512:(h + 1) * 512], in0=ps[0:nt, obk[h], :], scalar=stat[0:nt, 28 + h:29 + h],
                        in1=RA[0:nt, (16 + 4 * c + h) * 512:(17 + 4 * c + h) * 512], op0=ALU.mult, op1=ALU.mult),
                         reads=[PB(obk[h]), B("rso"), B("RA", 16 + 4 * c + h)], writes=[B("og", h)])
                if last and not sample:
                    tok = P.dma("sp", sp_o[seq_idx].rearrange("h (kk p) e -> p (h kk) e", p=128), Sf[:], lane_so,
                                reads=[B("Sf", i) for i in range(8)])
                    out_tokens.append(tok)
                tb = nb(2, 4, 8)
                for f in range(16):
                    bk = tb[f // 8]
                    psb = ps[:, bk, :].bitcast(BF16)
                    P.op("pe", lambda e, f=f, psb=psb: e.transpose(psb[:, (f % 8) * nt:(f % 8 + 1) * nt], og[0:nt, f * 128:(f + 1) * 128], ident[0:nt, 0:nt]),
                         reads=[B("og", f // 4)] + CB, writes=[PB(bk)], signal=(f % 8 == 7))
                for half in range(2):
                    bk = tb[half]
                    psb = ps[:, bk, :].bitcast(BF16)
                    P.op("act", lambda e, half=half, psb=psb, cs=cs: e.activation(
                        out=QKO[:, half * 8:(half + 1) * 8, cs], in_=psb[:, 0:8 * nt].rearrange("p (k t) -> p k t", k=8), func=AF.Copy),
                         reads=[PB(bk)], writes=[B("QKO", half * 8 + k, c) for k in range(8)])
            if os.environ.get("DBG"):
                ld = P.lane()
                out_tokens.append(P.dma("pool", ys[0:64, :], RA[0:64, 0:1024], ld, reads=[B("RA", 0), B("RA", 1)]))
                ld2 = P.lane()
                out_tokens.append(P.dma("pool", kws[0], khat[:, 1, 0:256], ld2, reads=[B("khat", 1, 0)]))
                if not seq_last:
                    ld3 = P.lane()
                    out_tokens.append(P.dma("sp", sp_o[seq_idx].rearrange("h (kk p) e -> p (h kk) e", p=128), Sf[:], ld3,
                                            reads=[B("Sf", i) for i in range(8)]))
            if RSTOP < 4:
                return
            dense_out_phase_named(nt, nch, QKO, lambda fk, c: B("QKO", fk, c), 2, 0)

        lane_sf = [P.lane(), P.lane()]
        lane_so2 = [P.lane(), P.lane()]

        def qx_base(h, kk):
            return (4 if h < 2 else 20) * 512 + ((h % 2) * 2 + kk) * 1024

        def sample_cross(obk):
            nt = NSTOK
            for h in range(4):
                for kk in range(2):
                    base = qx_base(h, kk)
                    u0 = base // 512
                    P.op("pool", lambda e, h=h, kk=kk, base=base: e.tensor_tensor(
                        out=RA[:, base:base + NSB * nt].rearrange("p (b t) -> p b t", b=NSB),
                        in0=QKO[:, 2 * h + kk, 0:nt].unsqueeze(1).to_broadcast([128, NSB, nt]), in1=bmq_t[:], op=ALU.mult),
                         reads=[B("QKO", 2 * h + kk, 0)] + CB, writes=[B("RA", u0), B("RA", u0 + 1)])
            for b in range(NSB):
                i2 = b % 2
                P.op("dve", lambda e, b=b, i2=i2: e.tensor_scalar(og[0:nt, i2 * 1024:(i2 + 1) * 1024], khat[0:nt, 0, :],
                                                                   stat[0:nt, 48 + b:49 + b], None, op0=ALU.mult),
                     reads=[B("khat", 0, h) for h in range(4)] + CB, writes=[B("og", 2 * i2), B("og", 2 * i2 + 1)])
                for hp in range(2):
                    sfb = [B("Sf", 4 * hp + i) for i in range(4)]
                    P.dma("sp", Sf[:, 4 * hp:4 * hp + 4, :], st_in[b, 2 * hp:2 * hp + 2].rearrange("h (kk p) e -> p (h kk) e", p=128),
                          lane_sf[hp], writes=sfb)
                    for i in range(4):
                        si_ = 4 * hp + i
                        if i % 2 == 0:
                            P.op("act", lambda e, si_=si_: e.activation(out=Sb[:, si_, :], in_=Sf[:, si_, :], func=AF.Copy),
                                 reads=[B("Sf", si_)], writes=[B("Sb", si_)])
                        else:
                            P.op("pool", lambda e, si_=si_: e.tensor_copy(Sb[:, si_, :], Sf[:, si_, :]),
                                 reads=[B("Sf", si_)], writes=[B("Sb", si_)])
                    for hh in range(2):
                        h = 2 * hp + hh
                        for kk in range(2):
                            si_ = 2 * h + kk
                            base = qx_base(h, kk) + b * nt
                            last = (b == NSB - 1 and kk == 1)
                            P.op("pe", lambda e, h=h, si_=si_, base=base, last=last: e.matmul(
                                ps[0:nt, obk[h], :], lhsT=RA[:, base:base + nt], rhs=Sb[:, si_, :], start=False, stop=last),
                                 reads=[B("RA", base // 512), B("Sb", si_)], writes=[PB(obk[h])])
                            (sbk,) = nb(1, 4, 8)
                            co = i2 * 1024 + h * 256 + kk * 128
                            P.op("pe", lambda e, h=h, sbk=sbk, co=co: e.matmul(
                                ps[:, sbk, :], lhsT=og[0:nt, co:co + 128], rhs=RA[0:nt, h * 512:(h + 1) * 512], start=True, stop=True),
                                 reads=[B("og", co // 512), B("RA", h)], writes=[PB(sbk)])
                            P.op("dve", lambda e, sbk=sbk, si_=si_, h=h: e.scalar_tensor_tensor(
                                out=Sf[:, si_, :], in0=Sf[:, si_, :], scalar=float(gdec[h] ** 4), in1=ps[:, sbk, :],
                                op0=ALU.mult, op1=ALU.add),
                                 reads=[PB(sbk), B("Sf", si_)], writes=[B("Sf", si_)])
                    out_tokens.append(P.dma("sp", ss_o[b, 2 * hp:2 * hp + 2].rearrange("h (kk p) e -> p (h kk) e", p=128),
                                            Sf[:, 4 * hp:4 * hp + 4, :], lane_so2[hp], reads=sfb))

        def swa_layer(nt, nch, sample, seq_first, seq_last, seq_idx, chunk0):
            TT = nt * nch
            import os
            norm_phase(nt, nch, [(2, xTa, "xTa"), (3, xTb, "xTb")])
            wt, wb, g = w_get()

            def kv_mm(c):
                (bk,) = nb()
                for k in range(8):
                    P.op("pe", lambda e, k=k, c=c, bk=bk, wt=wt: e.matmul(
                        ps[0:nt, bk, :], lhsT=xTa[:, k, c * nt:(c + 1) * nt], rhs=wt[:, k, :], start=(k == 0), stop=(k == 7)),
                         reads=[B("xTa", c), wb], writes=[PB(bk)], signal=(k == 7))
                return bk

            def kv_post(c, bk):
                slot = c + 1
                i2 = c % 2
                kf, vf, kb = kf2[:, i2, :], vf2[:, i2, :], kb2[:, i2, :]
                P.op("act", lambda e: e.activation(out=kf[0:nt, :], in_=ps[0:nt, bk, 0:256], func=AF.Copy),
                     reads=[PB(bk)], writes=[B("kf", i2)])
                P.op("act", lambda e: e.activation(out=vf[0:nt, :], in_=ps[0:nt, bk, 256:512], func=AF.Copy),
                     reads=[PB(bk)], writes=[B("vf", i2)])
                P.op("dve", lambda e: e.tensor_copy(vaug[0:nt, slot, :, 0:64], ps[0:nt, bk, 256:512].rearrange("p (h d) -> p h d", h=4)),
                     reads=[PB(bk)], writes=[B("vaug", slot)])
                if sample:
                    rc, rsn = ropeAs[0:nt, 0, :], ropeAs[0:nt, 1, :]
                else:
                    rc, rsn = ropeA[0:nt, chunk0 + c, 0, :], ropeA[0:nt, chunk0 + c, 1, :]
                rope_tok(kf, 4, nt, rc, rsn, ("kf", i2))
                P.op("pool", lambda e: e.tensor_copy(kb[0:nt, :], kf[0:nt, :]), reads=[B("kf", i2)], writes=[B("kb", i2)])
                (tbk,) = nb()
                psb = ps[:, tbk, :].bitcast(BF16)
                for kv in range(4):
                    P.op("pe", lambda e, kv=kv: e.transpose(psb[0:64, kv * nt:(kv + 1) * nt], kb[0:nt, kv * 64:(kv + 1) * 64], ident[0:nt, 0:nt]),
                         reads=[B("kb", i2)] + CB, writes=[PB(tbk)], signal=(kv == 3))
                P.op("dve", lambda e: e.tensor_copy(kTs[:, :, slot * 128:slot * 128 + nt],
                                                    psb[0:64, 0:4 * nt].rearrange("p (h t) -> p h t", h=4)),
                     reads=[PB(tbk)], writes=[B("kTs", slot)])
                if seq_last and c == nch - 1 and not sample:
                    out_tokens.append(P.dma("sp", kwp[seq_idx], kf[:, :], lane_kv, reads=[B("kf", i2)]))
                    out_tokens.append(P.dma("sp", vwp[seq_idx], vf[:, :], lane_vv, reads=[B("vf", i2)]))
                if sample:
                    for b in range(NSB):
                        out_tokens.append(P.dma("sp", kws[b, 124:128, :], kf[4 * b:4 * b + 4, :], lane_kv, reads=[B("kf", i2)]))
                        out_tokens.append(P.dma("sp", vws[b, 124:128, :], vf[4 * b:4 * b + 4, :], lane_vv, reads=[B("vf", i2)]))

            kvb = {0: kv_mm(0)}
            for c in range(1, nch):
                kvb[c] = kv_mm(c)
                kv_post(c - 1, kvb[c - 1])
            w_done(g)
            kv_post(nch - 1, kvb[nch - 1])
            import os
            SSTOP = int(os.environ.get("SSTOP", "99"))
            if SSTOP < 1:
                return
            qTs = QKO[0:64, :, :]

            def q_mm(ch, c, wt, wb):
                (bk,) = nb()
                for k in range(8):
                    P.op("pe", lambda e, k=k, c=c, wt=wt, bk=bk: e.matmul(
                        ps[0:nt, bk, :], lhsT=xTb[:, k, c * nt:(c + 1) * nt], rhs=wt[:, k, :], start=(k == 0), stop=(k == 7)),
                         reads=[B("xTb", c), wb], writes=[PB(bk)], signal=(k == 7))
                return bk

            def q_post(ch, c, bk, gi_):
                i2 = gi_ % 2
                qfv = qf[:, i2 * 512:(i2 + 1) * 512]
                qbv = qb[:, i2 * 512:(i2 + 1) * 512]
                P.op("act", lambda e: e.activation(out=qfv[0:nt, :], in_=ps[0:nt, bk, :], func=AF.Copy),
                     reads=[PB(bk)], writes=[B("qf", i2)])
                if sample:
                    rc, rsn = ropeAs[0:nt, 0, :], ropeAs[0:nt, 1, :]
                else:
                    rc, rsn = ropeA[0:nt, chunk0 + c, 0, :], ropeA[0:nt, chunk0 + c, 1, :]
                rope_tok(qfv, 8, nt, rc, rsn, ("qf", i2))
                P.op("pool", lambda e: e.tensor_copy(qbv[0:nt, :], qfv[0:nt, :]), reads=[B("qf", i2)], writes=[B("qb", i2)])
                (tbk,) = nb()
                psb = ps[:, tbk, :].bitcast(BF16)
                for hh in range(8):
                    P.op("pe", lambda e, hh=hh: e.transpose(psb[0:64, hh * nt:(hh + 1) * nt], qbv[0:nt, hh * 64:(hh + 1) * 64], ident[0:nt, 0:nt]),
                         reads=[B("qb", i2)] + CB, writes=[PB(tbk)], signal=(hh == 7))
                P.op("dve", lambda e: e.tensor_copy(
                    qTs[:, ch * 8:(ch + 1) * 8, c * nt:(c + 1) * nt], psb[0:64, 0:8 * nt].rearrange("p (h t) -> p h t", h=8)),
                     reads=[PB(tbk)], writes=[B("QKO", ch * 8 + hh, c) for hh in range(8)])

            pend = None
            gi_ = 0
            for ch in range(2):
                wt, wb, g = w_get()
                for c in range(nch):
                    bk = q_mm(ch, c, wt, wb)
                    if pend is not None:
                        q_post(*pend)
                    pend = (ch, c, bk, gi_)
                    gi_ += 1
                w_done(g)
            q_post(*pend)
            if SSTOP < 2:
                return
            for c in range(nch):
                if sample:
                    sample_attention()
                    break
                first = seq_first and c == 0
                blks = [1] if first else [0, 1]
                for kv in range(4):
                    for bl in blks:
                        kslot = c + bl
                        (bk,) = nb()
                        P.op("pe", lambda e, kv=kv, kslot=kslot, bk=bk, c=c: e.matmul(
                            ps[:, bk, :].rearrange("p (h t) -> p h t", h=4), lhsT=kTs[:, kv, kslot * 128:(kslot + 1) * 128],
                            rhs=qTs[:, 4 * kv:4 * kv + 4, c * 128:(c + 1) * 128], start=True, stop=False),
                             reads=[B("kTs", kslot)] + [B("QKO", 4 * kv + i, c) for i in range(4)], writes=[PB(bk)], signal=False)
                        P.op("pe", lambda e, bl=bl, bk=bk: e.matmul(ps[:, bk, :], lhsT=ident[:], rhs=amask_t[:, bl, :], start=False, stop=True),
                             reads=CB, writes=[PB(bk)])
                        u = 4 * bl + kv
                        P.op("act", lambda e, bk=bk, u=u: e.activation(out=RA[:, u * 512:(u + 1) * 512], in_=ps[:, bk, :], func=AF.Exp, scale=0.125),
                             reads=[PB(bk)], writes=[B("RA", u)])
                obk = nb(4)
                for h in range(16):
                    kv = h // 4
                    for bi_, bl in enumerate(blks):
                        kslot = c + bl
                        u = 4 * bl + kv
                        P.op("pe", lambda e, h=h, kv=kv, u=u, kslot=kslot, bi_=bi_, nbl=len(blks), ob4=obk[h // 4]: e.matmul(
                            ps[:, ob4, (h % 4) * 128:(h % 4) * 128 + 65],
                            lhsT=RA[:, u * 512 + (h % 4) * 128:u * 512 + (h % 4 + 1) * 128], rhs=vaug[:, kslot, kv, :],
                            start=(bi_ == 0), stop=(bi_ == nbl - 1)),
                             reads=[B("RA", u), B("vaug", kslot)], writes=[PB(obk[h // 4])], signal=(bi_ == len(blks) - 1 and h % 4 == 3))
                attn_finish(nt, c, obk)
            if SSTOP < 3:
                return
            if not sample and not seq_last:
                P.op("pool", lambda e: e.tensor_copy(kTs[:, :, 0:128], kTs[:, :, 512:640]), reads=[B("kTs", 4)], writes=[B("kTs", 0)])
                P.op("pool", lambda e: e.tensor_copy(vaug[:, 0, :, :], vaug[:, 4, :, :]), reads=[B("vaug", 4)], writes=[B("vaug", 0)])
            dense_out_phase_named(nt, nch, xTa, lambda fk, c: B("xTa", c), 1, 2)

        def attn_finish(nt, c, obk):
            den = stat[0:nt, 32:48]
            for q4 in range(4):
                P.op("dve", lambda e, q4=q4: e.tensor_tensor(
                    out=stat[0:nt, 32 + 4 * q4:36 + 4 * q4], in0=ps[0:nt, obk[q4], :].rearrange("p (h d) -> p h d", h=4)[:, :, 64],
                    in1=sexp_t[0:nt, 4 * q4:4 * q4 + 4], op=ALU.add),
                     reads=[PB(obk[q4])] + CB, writes=[B("den", q4)])
            P.op("dve", lambda e: e.reciprocal(den, den), reads=[B("den", i) for i in range(4)], writes=[B("rden")])
            for q4 in range(4):
                P.op("dve", lambda e, q4=q4: e.tensor_tensor(
                    out=ob[0:nt, q4 * 256:(q4 + 1) * 256].rearrange("p (h d) -> p h d", h=4),
                    in0=ps[0:nt, obk[q4], :].rearrange("p (h d) -> p h d", h=4)[:, :, 0:64],
                    in1=stat[0:nt, 32 + 4 * q4:36 + 4 * q4].unsqueeze(2).to_broadcast([nt, 4, 64]), op=ALU.mult),
                     reads=[PB(obk[q4]), B("rden")], writes=[B("ob")])
            (tbk,) = nb()
            psb = ps[:, tbk, :].bitcast(BF16)
            for k in range(8):
                P.op("pe", lambda e, k=k, psb=psb: e.transpose(psb[:, k * nt:(k + 1) * nt], ob[0:nt, k * 128:(k + 1) * 128], ident[0:nt, 0:nt]),
                     reads=[B("ob")] + CB, writes=[PB(tbk)], signal=(k == 7))
            P.op("act", lambda e, psb=psb, c=c: e.activation(out=xTa[:, :, c * nt:(c + 1) * nt],
                                                             in_=psb[:, 0:8 * nt].rearrange("p (k t) -> p k t", k=8), func=AF.Copy),
                 reads=[PB(tbk)], writes=[B("xTa", c)])

        def rope_tok(buf, nh, nt, rc, rsn, bkey):
            v = buf[0:nt, 0:nh * 64].rearrange("p (h d) -> p h d", h=nh)
            x1, x2 = v[:, :, 0:8], v[:, :, 8:16]
            cb = rc.unsqueeze(1).to_broadcast([nt, nh, 8])
            sbb = rsn.unsqueeze(1).to_broadcast([nt, nh, 8])
            t = [atmp[0:nt, i, 0:nh, :] for i in range(4)]
            bb_ = [B(*bkey), B("atmp")] + CB
            P.op("dve", lambda e: e.tensor_tensor(out=t[0], in0=x1, in1=cb, op=ALU.mult), reads=bb_, writes=[B("atmp")])
            P.op("dve", lambda e: e.tensor_tensor(out=t[1], in0=x2, in1=sbb, op=ALU.mult), reads=bb_, writes=[B("atmp")])
            P.op("dve", lambda e: e.tensor_tensor(out=t[2], in0=x2, in1=cb, op=ALU.mult), reads=bb_, writes=[B("atmp")])
            P.op("dve", lambda e: e.tensor_tensor(out=t[3], in0=x1, in1=sbb, op=ALU.mult), reads=bb_, writes=[B("atmp")])
            P.op("dve", lambda e: e.tensor_tensor(out=x1, in0=t[0], in1=t[1], op=ALU.subtract), reads=[B("atmp")], writes=[B(*bkey)])
            P.op("dve", lambda e: e.tensor_tensor(out=x2, in0=t[2], in1=t[3], op=ALU.add), reads=[B("atmp")], writes=[B(*bkey)])

        lane_ck = [P.lane(), P.lane()]
        lane_cv = [P.lane(), P.lane()]
        lane_am = [P.lane(), P.lane()]
        lane_kw = [P.lane(), P.lane()]
        lane_vw = [P.lane(), P.lane()]

        def sample_attention():
            nt = NSTOK
            qTs = QKO[0:64, :, :]
            vc = Sf[:].rearrange("p a b -> p (a b)").bitcast(BF16)[:, 0:NSB * 260].rearrange("p (b h d) -> p b h d", b=NSB, h=4)
            P.op("pool", lambda e: e.memset(Sf[:].rearrange("p a b -> p (a b)").bitcast(BF16)[:, 0:NSB * 260], 1.0),
                 writes=[B("Sf", i) for i in range(8)])
            for kvp in range(2):
                (bk,) = nb()
                for kvi in range(2):
                    kv = 2 * kvp + kvi
                    P.op("pe", lambda e, kv=kv, kvi=kvi, bk=bk: e.matmul(
                        ps[0:nt, bk, kvi * 256:(kvi + 1) * 256].rearrange("p (h t) -> p h t", h=4), lhsT=kTs[:, kv, 128:128 + nt],
                        rhs=qTs[:, 4 * kv:4 * kv + 4, 0:nt], start=True, stop=False),
                         reads=[B("kTs", 1)] + [B("QKO", 4 * kv + i, 0) for i in range(4)], writes=[PB(bk)], signal=False)
                    P.op("pe", lambda e, kvi=kvi, bk=bk: e.matmul(ps[0:nt, bk, kvi * 256:(kvi + 1) * 256], lhsT=ident[0:nt, 0:nt],
                                                                  rhs=amask_t[0:nt, 0, 256:512], start=False, stop=True),
                         reads=CB, writes=[PB(bk)])
                P.op("act", lambda e, bk=bk, kvp=kvp: e.activation(out=xn[0:nt, 0, kvp * 512:(kvp + 1) * 512], in_=ps[0:nt, bk, :],
                                                                   func=AF.Exp, scale=0.125),
                     reads=[PB(bk)], writes=[B("xn", 0)])
            for b in range(NSB):
                i2 = b % 2
                ckf = qf[:, i2 * 512:i2 * 512 + 256]
                cvf = qf[:, i2 * 512 + 256:i2 * 512 + 512]
                al = [B("qf", i2)] if b < 2 else []
                P.dma("sp", ckf, ck_in[b], lane_ck[i2], writes=[B("ckf", i2)] + al)
                P.dma("sp", cvf, cv_in[b], lane_cv[i2], writes=[B("cvf", i2)] + al)
                P.dma("sp", amask_t[:, i2, 0:256], T["amask_c"][b], lane_am[i2], writes=[B("amc", i2)])
                out_tokens.append(P.dma("sp", kws[b, 0:124, :], qf[4:128, i2 * 512:i2 * 512 + 256], lane_kw[i2], reads=[B("ckf", i2)]))
                out_tokens.append(P.dma("sp", vws[b, 0:124, :], qf[4:128, i2 * 512 + 256:i2 * 512 + 512], lane_vw[i2], reads=[B("cvf", i2)]))
                P.op("pool", lambda e, i2=i2, ckf=ckf: e.tensor_copy(qb[:, i2 * 256:(i2 + 1) * 256], ckf),
                     reads=[B("ckf", i2)], writes=[B("qbc", i2)] + ([B("qb", 0)] if b < 2 else []))
                (tbk,) = nb()
                psb = ps[:, tbk, :].bitcast(BF16)
                for kv in range(4):
                    P.op("pe", lambda e, kv=kv, psb=psb, i2=i2: e.transpose(psb[0:64, kv * 128:(kv + 1) * 128],
                                                                            qb[:, i2 * 256 + kv * 64:i2 * 256 + (kv + 1) * 64], ident[:]),
                         reads=[B("qbc", i2)] + CB, writes=[PB(tbk)], signal=(kv == 3))
                P.op("dve", lambda e, psb=psb, i2=i2: e.tensor_copy(kTs[:, :, 256 + i2 * 128:256 + (i2 + 1) * 128],
                                                                    psb[0:64, 0:512].rearrange("p (h t) -> p h t", h=4)),
                     reads=[PB(tbk)], writes=[B("kTc", i2)] + ([B("kTs", 2 + i2)] if b < 2 else []))
                P.op("pool", lambda e, b=b, cvf=cvf: e.tensor_copy(vc[:, b, :, 0:64], cvf.rearrange("p (h d) -> p h d", h=4)),
                     reads=[B("cvf", i2)] + [B("Sf", i) for i in range(8)], writes=[B("vc", b)])
                for kvp in range(2):
                    (bk,) = nb()
                    for kvi in range(2):
                        kv = 2 * kvp + kvi
                        P.op("pe", lambda e, kv=kv, kvi=kvi, bk=bk, i2=i2: e.matmul(
                            ps[:, bk, kvi * 256:(kvi + 1) * 256].rearrange("p (h t) -> p h t", h=4),
                            lhsT=kTs[:, kv, 256 + i2 * 128:256 + (i2 + 1) * 128], rhs=qTs[:, 4 * kv:4 * kv + 4, 0:nt], start=True, stop=False),
                             reads=[B("kTc", i2)] + [B("QKO", 4 * kv + i, 0) for i in range(4)], writes=[PB(bk)], signal=False)
                        P.op("pe", lambda e, kvi=kvi, bk=bk, i2=i2: e.matmul(ps[:, bk, kvi * 256:(kvi + 1) * 256], lhsT=ident[:],
                                                                             rhs=amask_t[:, i2, 0:256], start=False, stop=True),
                             reads=[B("amc", i2)] + CB, writes=[PB(bk)])
                    u = 2 * b + kvp
                    P.op("act", lambda e, bk=bk, u=u: e.activation(out=RA[:, u * 512:(u + 1) * 512], in_=ps[:, bk, :], func=AF.Exp, scale=0.125),
                         reads=[PB(bk)], writes=[B("RA", u)])
            obk = nb(4)
            for h in range(16):
                kv = h // 4
                bank = obk[h // 4]
                for b in range(NSB):
                    co = (2 * b + kv // 2) * 512 + (kv % 2) * 256 + (h % 4) * 64
                    P.op("pe", lambda e, h=h, b=b, kv=kv, co=co, bank=bank: e.matmul(
                        ps[0:nt, bank, (h % 4) * 128:(h % 4) * 128 + 65], lhsT=RA[:, co:co + nt], rhs=vc[:, b, kv, :],
                        start=(b == 0), stop=False),
                         reads=[B("RA", co // 512), B("vc", b)], writes=[PB(bank)], signal=False)
                P.op("pe", lambda e, h=h, kv=kv, bank=bank: e.matmul(
                    ps[0:nt, bank, (h % 4) * 128:(h % 4) * 128 + 65], lhsT=xn[0:nt, 0, h * 64:(h + 1) * 64], rhs=vaug[0:nt, 1, kv, :],
                    start=False, stop=True),
                     reads=[B("xn", 0), B("vaug", 1)], writes=[PB(bank)])
            attn_finish(nt, 0, obk)

        tile_no = 0
        for sq in range(NSEQ):
            for ti in range(NTPS):
                r0 = sq * SEQ + ti * 512
                ri = tile_no % 2
                P.dma("sp", ropeR[:, ri], T["rope_r"][ti], lane_rope[ri], writes=[B("rope")])
                for c in range(4):
                    P.dma("sp", hb[:, c, :], xp[r0 + c * 128:r0 + (c + 1) * 128, :], lane_h[c], writes=[B("h", c)])
                import os
                KSTOP = int(os.environ.get("KSTOP", "99"))
                if KSTOP >= 1:
                    ret_layer(128, 4, False, ti == 0, ti == NTPS - 1, sq, ropeR[:, ri], dq_t, maskr_t, kdec_t)
                if KSTOP >= 2:
                    ffn_phase(128, 4, 0, 1, 1)
                if KSTOP >= 3:
                    swa_layer(128, 4, False, ti == 0, ti == NTPS - 1, sq, ti * 4)

                def store(c, r0=r0):
                    out_tokens.append(P.dma("sp", yp[r0 + c * 128:r0 + (c + 1) * 128, :], hb[:, c, :], lane_y[c], reads=[B("h", c)]))

                if KSTOP >= 4:
                    ffn_phase(128, 4, 1, 4, 3, after_chunk=store)
                else:
                    for c in range(4):
                        store(c)
                tile_no += 1
                if tile_no >= int(os.environ.get("KTILES", "99")):
                    break
            if tile_no >= int(os.environ.get("KTILES", "99")):
                break

        if do_sample:
            nts = NSTOK
            P.dma("sp", dq_t[:, :, 0:nts], T["dqs"], lane_c, writes=[B("c")])
            P.dma("sp", maskr_t[0:nts, :, 0:nts], T["maskrs"], lane_c, writes=[B("c")])
            P.dma("sp", kdec_t[0:nts, :], T["kdecs"], lane_c, writes=[B("c")])
            P.dma("sp", amask_t[0:nts, 0, 256:512], T["amask_n"], lane_c, writes=[B("c")])
            P.dma("sp", bmq_t[:], T["bmq"], lane_c, writes=[B("c")])
            P.dma("sp", stat[0:nts, 48:48 + NSB], T["bmrow"], lane_c, writes=[B("c")])
            P.dma("sp", ropeAs[0:nts], T["rope_as"], lane_c, writes=[B("c")])
            ri = tile_no % 2
            P.dma("sp", ropeR[:, ri, :, 0:nts], T["rope_rs"], lane_rope[ri], writes=[B("rope")])
            P.dma("sp", hb[0:nts, 0, :], xs, lane_h[0], writes=[B("h", 0)])
            ret_layer(nts, 1, True, False, False, 0, ropeR[:, ri], dq_t[:, :, 0:nts], maskr_t, kdec_t)

            def store_s(c):
                out_tokens.append(P.dma("sp", ys[:, :], hb[0:nts, 0, :], lane_y[0], reads=[B("h", 0)]))

            DBGS = int(os.environ.get("DBGS", "0"))
            if DBGS == 1:
                store_s(0)
            else:
                ffn_phase(nts, 1, 0, 1, 1)
                if DBGS == 2:
                    store_s(0)
                else:
                    swa_layer(nts, 1, True, True, True, 0, 0)
                    ffn_phase(nts, 1, 1, 4, 3, after_chunk=store_s)

        P.wait_tokens("sp", out_tokens)
        P.wait_tokens("sp", [(l.key, l.val) for l in lanes_ring + lanes_ring_sw + lanes_scr + lane_h + lane_y + lane_rope + lane_sf + lane_so2 + lane_ck + lane_cv + lane_am + lane_kw + lane_vw + [lane_c, lane_g, lane_so, lane_kv, lane_vv] if l.val > 0])
        P.emit(block)
    return nc


def kernel(x_prompt, x_sample, state_ret, cache_k_win, cache_v_win,
           ret_norm_pre, ret_w_in, ret_w_out, ret_norm_post, kv_norm, w_kv,
           swa_norm_pre, swa_w_q, swa_sinks, swa_w_o, swa_norm_post,
           ffn_norm_pre, ffn_w1, ffn_w2, ffn_norm_post, _ncores=8, _do_sample=True):
    f = lambda a: np.ascontiguousarray(np.asarray(a, dtype=np.float32))
    x_prompt, x_sample = f(x_prompt), f(x_sample)
    BATCH, SEQ, D = x_prompt.shape
    DB = x_sample.shape[0]
    n = _ncores
    NSEQ = BATCH // n
    NSB = DB // n
    tabs = host_tables(SEQ, NSB)
    nc = build(NSEQ, SEQ, NSB, tabs, do_sample=_do_sample)
    gcol = np.stack([f(ret_norm_pre)[0], f(ffn_norm_pre)[0], f(kv_norm), f(swa_norm_pre)[0], f(ffn_norm_pre)[1],
                     f(ffn_norm_pre)[1]], 0)
    grow = np.stack([np.broadcast_to(v, (128, 1024)) for v in
                     (f(ret_norm_post)[0], f(ffn_norm_post)[0], f(swa_norm_post)[0], f(ffn_norm_post)[1])], 0)
    shared = {
        "w_in": f(ret_w_in)[0], "w_out": f(ret_w_out)[0], "w_kv": f(w_kv), "w_q": f(swa_w_q)[0], "w_o": f(swa_w_o)[0],
        "w1": f(ffn_w1), "w2": f(ffn_w2), "gcol": np.ascontiguousarray(gcol), "grow": np.ascontiguousarray(grow),
        "sinks": np.ascontiguousarray(np.broadcast_to(f(swa_sinks)[0], (128, 16))),
    }
    for k, v in tabs.items():
        shared["t_" + k] = np.ascontiguousarray(v)
    st = f(state_ret)[0]
    ck, cv = f(cache_k_win), f(cache_v_win)
    in_maps = []
    for i in range(n):
        m = dict(shared)
        m["xp"] = x_prompt[i * NSEQ:(i + 1) * NSEQ].reshape(NSEQ * SEQ, D)
        m["xs"] = x_sample[i * NSB:(i + 1) * NSB].reshape(NSB * 4, D)
        m["st_in"] = st[i * NSB:(i + 1) * NSB]
        m["ck_in"] = ck[i * NSB:(i + 1) * NSB].reshape(NSB, 128, 256)
        m["cv_in"] = cv[i * NSB:(i + 1) * NSB].reshape(NSB, 128, 256)
        in_maps.append(m)
    res = run_bass_kernel_spmd(nc, in_maps, core_ids=list(range(n)))
    R = res.results
    cat = lambda k: np.concatenate([np.asarray(r[k]) for r in R], 0)
    y_prompt = cat("yp").reshape(BATCH, SEQ, D)
    y_sample = cat("ys").reshape(DB, 4, D)
    sp_ = cat("sp_o")[None]
    ss_ = cat("ss_o")[None]
    kwp_ = cat("kwp").reshape(BATCH, 128, 4, 64)
    vwp_ = cat("vwp").reshape(BATCH, 128, 4, 64)
    kws_ = cat("kws").reshape(DB, 128, 4, 64)
    vws_ = cat("vws").reshape(DB, 128, 4, 64)
    return (y_prompt, y_sample, sp_, ss_, kwp_, vwp_, kws_, vws_)
```
